# Optimizing a Trainium2 kernel written in Bass

```python
import math
import jax
import jax.numpy as jnp
from jax import lax
import numpy as np

D_MODEL = 1024
BATCH = 4
SEQ = 8192
DEPTH = 2

GRID_W = 64
CTX_LEN = 256
N_MIXERS = 2
HEAD_DIM = 64
A_HEADS = 16
A_KV_HEADS = 4
A_WINDOW = 128
A_BLOCK = 128
B_HEADS = 16
NA_ROWS = 8
NA_COLS = 16
NA_QCOLS = 16
NA_KCOLS = 32
N_EXPERTS = 16
EXPERT_FF = 2048
EC_CAPACITY = 2
ROPE_BASE = 10000.0
EPS = 1e-6
NEG_INF = -1e30
ATTN_SCALE = HEAD_DIM ** -0.5

kernel_name = 'hybrid_swa_natten_ecmoe_dit'


def rms_norm(x, g):
    xf = x.astype(jnp.float32)
    y = xf * lax.rsqrt(jnp.mean(xf * xf, axis=-1, keepdims=True) + EPS)
    return (y * g.astype(jnp.float32)).astype(x.dtype)


def modulate(h, shift, scale):
    return h * (1 + scale[:, None]) + shift[:, None]


def axial_rope_tables(n_tok):
    pos = jnp.arange(n_tok)
    row = (pos // GRID_W).astype(jnp.float32)
    col = (pos % GRID_W).astype(jnp.float32)
    n_freq = HEAD_DIM // 4
    inv = ROPE_BASE ** (-jnp.arange(n_freq, dtype=jnp.float32) / n_freq)
    ang_r = row[:, None] * inv
    ang_c = col[:, None] * inv
    return (jnp.cos(ang_r), jnp.sin(ang_r), jnp.cos(ang_c), jnp.sin(ang_c))


def axial_rope(x, tabs):
    cos_r, sin_r, cos_c, sin_c = tabs
    S = x.shape[1]

    def rot(v, cs, sn):
        shape = (1, S) + (1,) * (v.ndim - 3) + (cs.shape[-1],)
        cs = cs.reshape(shape)
        sn = sn.reshape(shape)
        v1, v2 = jnp.split(v.astype(jnp.float32), 2, axis=-1)
        return jnp.concatenate([v1 * cs - v2 * sn, v1 * sn + v2 * cs], axis=-1)

    xr, xc = jnp.split(x, 2, axis=-1)
    return jnp.concatenate([rot(xr, cos_r, sin_r), rot(xc, cos_c, sin_c)], axis=-1).astype(x.dtype)


def sink_softmax(s, sink):
    m = jnp.maximum(jnp.max(s, axis=-1, keepdims=True), sink)
    p = jnp.exp(s - m)
    return p / (jnp.sum(p, axis=-1, keepdims=True) + jnp.exp(sink - m))


def window_gqa_sink(hl, hc, w_qkv, q_gain, k_gain, sink, w_o, rope_tabs, ctx_out):
    B, S, _ = hl.shape
    G = A_HEADS // A_KV_HEADS
    nb = S // A_BLOCK
    cuts = [A_HEADS * HEAD_DIM, (A_HEADS + A_KV_HEADS) * HEAD_DIM]

    def project(h):
        n = h.shape[1]
        q, k, v = jnp.split(h @ w_qkv, cuts, axis=-1)
        q = rms_norm(q.reshape(B, n, A_KV_HEADS, G, HEAD_DIM), q_gain)
        k = rms_norm(k.reshape(B, n, A_KV_HEADS, HEAD_DIM), k_gain)
        return q, k, v.reshape(B, n, A_KV_HEADS, HEAD_DIM)

    ql, kl, vl = project(hl)
    qc, kc, vc = project(hc)
    ql = axial_rope(ql, rope_tabs)
    kl = axial_rope(kl, rope_tabs)
    sink_l = sink.astype(jnp.float32).reshape(1, A_KV_HEADS, G, 1, 1)

    pad = ((0, 0), (A_BLOCK, A_BLOCK), (0, 0), (0, 0))
    kpad = jnp.pad(kl, pad)
    vpad = jnp.pad(vl, pad)
    qb = jnp.moveaxis(ql.reshape(B, nb, A_BLOCK, A_KV_HEADS, G, HEAD_DIM), 1, 0)
    span = 3 * A_BLOCK
    rel = jnp.arange(A_BLOCK)[:, None] - (jnp.arange(span)[None, :] - A_BLOCK)
    in_window = jnp.abs(rel) <= A_WINDOW

    def block(args):
        i, q = args
        start = i * A_BLOCK
        kw = lax.dynamic_slice_in_dim(kpad, start, span, axis=1)
        vw = lax.dynamic_slice_in_dim(vpad, start, span, axis=1)
        kpos = start - A_BLOCK + jnp.arange(span)
        valid = in_window & ((kpos >= 0) & (kpos < S))[None, :]
        s_loc = jnp.einsum('bqhgd,bkhd->bhgqk', q, kw).astype(jnp.float32) * ATTN_SCALE
        s_loc = jnp.where(valid, s_loc, NEG_INF)
        s_ctx = jnp.einsum('bqhgd,bkhd->bhgqk', q, kc).astype(jnp.float32) * ATTN_SCALE
        p = sink_softmax(jnp.concatenate([s_loc, s_ctx], axis=-1), sink_l).astype(vl.dtype)
        return (jnp.einsum('bhgqk,bkhd->bqhgd', p[..., :span], vw)
                + jnp.einsum('bhgqk,bkhd->bqhgd', p[..., span:], vc))

    o = lax.map(block, (jnp.arange(nb), qb))
    yl = jnp.moveaxis(o, 0, 1).reshape(B, S, A_HEADS * HEAD_DIM) @ w_o
    if not ctx_out:
        return yl, None
    s_cc = jnp.einsum('bqhgd,bkhd->bhgqk', qc, kc).astype(jnp.float32) * ATTN_SCALE
    p_cc = sink_softmax(s_cc, sink_l).astype(vc.dtype)
    oc = jnp.einsum('bhgqk,bkhd->bqhgd', p_cc, vc).reshape(B, hc.shape[1], A_HEADS * HEAD_DIM)
    return yl, oc @ w_o


def neighbourhood_attention(hl, hc, w_qkv, q_gain, k_gain, rpb, w_o, ctx_out):
    B, S, _ = hl.shape
    R = S // GRID_W
    wr = min(NA_ROWS, R)
    nc = GRID_W // NA_QCOLS
    nk = wr * NA_KCOLS

    def project(h):
        n = h.shape[1]
        q, k, v = jnp.split(h @ w_qkv, 3, axis=-1)
        shp = (B, n, B_HEADS, HEAD_DIM)
        return rms_norm(q.reshape(shp), q_gain), rms_norm(k.reshape(shp), k_gain), v.reshape(shp)

    ql, kl, vl = project(hl)
    qc, kc, vc = project(hc)
    qg = jnp.moveaxis(ql.reshape(B, R, nc, NA_QCOLS, B_HEADS, HEAD_DIM), 1, 0)
    kg = kl.reshape(B, R, GRID_W, B_HEADS, HEAD_DIM)
    vg = vl.reshape(B, R, GRID_W, B_HEADS, HEAD_DIM)

    qcol = np.arange(nc)[:, None] * NA_QCOLS + np.arange(NA_QCOLS)[None, :]
    kstart = np.clip(np.arange(nc) * NA_QCOLS - (NA_KCOLS - NA_QCOLS) // 2, 0, GRID_W - NA_KCOLS)
    kcol = kstart[:, None] + np.arange(NA_KCOLS)[None, :]
    wstart = np.clip(qcol - NA_COLS // 2, 0, GRID_W - NA_COLS)
    kc3 = kcol[:, None, :]
    col_ok = (kc3 >= wstart[..., None]) & (kc3 < wstart[..., None] + NA_COLS)
    dcol_idx = np.clip(kc3 - qcol[..., None], -(NA_COLS - 1), NA_COLS - 1) + NA_COLS - 1
    mask = np.broadcast_to(col_ok[:, :, None, :], (nc, NA_QCOLS, wr, NA_KCOLS)).reshape(nc, NA_QCOLS, nk)
    rpb_cols = rpb.astype(jnp.float32)[:, :, dcol_idx]

    def row(args):
        r, q = args
        rs = jnp.clip(r - wr // 2, 0, R - wr)
        kr = lax.dynamic_slice_in_dim(kg, rs, wr, axis=1)[:, :, kcol]
        vr = lax.dynamic_slice_in_dim(vg, rs, wr, axis=1)[:, :, kcol]
        kr = jnp.transpose(kr, (0, 2, 1, 3, 4, 5)).reshape(B, nc, nk, B_HEADS, HEAD_DIM)
        vr = jnp.transpose(vr, (0, 2, 1, 3, 4, 5)).reshape(B, nc, nk, B_HEADS, HEAD_DIM)
        dr_idx = rs - r + jnp.arange(wr) + NA_ROWS - 1
        bias = jnp.transpose(rpb_cols[:, dr_idx], (0, 2, 3, 1, 4)).reshape(B_HEADS, nc, NA_QCOLS, nk)
        s_loc = jnp.einsum('bjqhd,bjkhd->bhjqk', q, kr).astype(jnp.float32) * ATTN_SCALE + bias
        s_loc = jnp.where(mask, s_loc, NEG_INF)
        s_ctx = jnp.einsum('bjqhd,bkhd->bhjqk', q, kc).astype(jnp.float32) * ATTN_SCALE
        p = jax.nn.softmax(jnp.concatenate([s_loc, s_ctx], axis=-1), axis=-1).astype(vl.dtype)
        return (jnp.einsum('bhjqk,bjkhd->bjqhd', p[..., :nk], vr)
                + jnp.einsum('bhjqk,bkhd->bjqhd', p[..., nk:], vc))

    o = lax.map(row, (jnp.arange(R), qg))
    yl = jnp.moveaxis(o, 0, 1).reshape(B, S, B_HEADS * HEAD_DIM) @ w_o
    if not ctx_out:
        return yl, None
    s_cc = jnp.einsum('bqhd,bkhd->bhqk', qc, kc).astype(jnp.float32) * ATTN_SCALE
    p_cc = jax.nn.softmax(s_cc, axis=-1).astype(vc.dtype)
    oc = jnp.einsum('bhqk,bkhd->bqhd', p_cc, vc).reshape(B, hc.shape[1], B_HEADS * HEAD_DIM)
    return yl, oc @ w_o


def ec_moe(x, w_router, w_gate, w_up, w_down):
    B, S, _ = x.shape
    cap = EC_CAPACITY * S // N_EXPERTS
    aff = jax.nn.softmax(jnp.einsum('bsd,de->bse', x, w_router).astype(jnp.float32), axis=-1)
    gate, idx = lax.top_k(jnp.swapaxes(aff, 1, 2), cap)
    bidx = jnp.arange(B)[:, None, None]
    xg = x[bidx, idx]
    h = jax.nn.silu(jnp.einsum('becd,edf->becf', xg, w_gate)) * jnp.einsum('becd,edf->becf', xg, w_up)
    y = jnp.einsum('becf,efd->becd', h, w_down) * gate[..., None].astype(x.dtype)
    return jnp.zeros_like(x).at[bidx, idx].add(y)


def setup_inputs(seed: int = 0) -> dict:
    key = jax.random.key(seed)
    ks = jax.random.split(key, 24)
    n_a = (DEPTH + N_MIXERS - 1) // N_MIXERS
    n_b = DEPTH // N_MIXERS
    f32 = jnp.float32

    def nrm(k, shape, scale):
        return jax.random.normal(k, shape, f32) * scale

    qkv_a = (A_HEADS + 2 * A_KV_HEADS) * HEAD_DIM
    d_a = A_HEADS * HEAD_DIM
    d_b = B_HEADS * HEAD_DIM
    return {
        'x': nrm(ks[0], (BATCH, SEQ, D_MODEL), 1.0),
        'c': nrm(ks[1], (BATCH, D_MODEL), 1.0),
        'ctx': nrm(ks[2], (BATCH, CTX_LEN, D_MODEL), 1.0),
        'c_ctx': nrm(ks[3], (D_MODEL,), 1.0),
        'w_mod': nrm(ks[4], (DEPTH, D_MODEL, 6 * D_MODEL), 0.5 * D_MODEL ** -0.5),
        'b_mod': nrm(ks[5], (DEPTH, 6 * D_MODEL), 0.02),
        'norm_mix': 1.0 + nrm(ks[6], (DEPTH, D_MODEL), 0.02),
        'norm_ffn': 1.0 + nrm(ks[7], (DEPTH, D_MODEL), 0.02),
        'a_w_qkv': nrm(ks[8], (n_a, D_MODEL, qkv_a), D_MODEL ** -0.5),
        'a_q_gain': 1.0 + nrm(ks[9], (n_a, HEAD_DIM), 0.02),
        'a_k_gain': 1.0 + nrm(ks[10], (n_a, HEAD_DIM), 0.02),
        'a_sink': nrm(ks[11], (n_a, A_HEADS), 0.5),
        'a_w_o': nrm(ks[12], (n_a, d_a, D_MODEL), d_a ** -0.5),
        'b_w_qkv': nrm(ks[13], (n_b, D_MODEL, 3 * d_b), D_MODEL ** -0.5),
        'b_q_gain': 1.0 + nrm(ks[14], (n_b, HEAD_DIM), 0.02),
        'b_k_gain': 1.0 + nrm(ks[15], (n_b, HEAD_DIM), 0.02),
        'b_rpb': nrm(ks[16], (n_b, B_HEADS, 2 * NA_ROWS - 1, 2 * NA_COLS - 1), 0.5),
        'b_w_o': nrm(ks[17], (n_b, d_b, D_MODEL), d_b ** -0.5),
        'moe_router': nrm(ks[18], (DEPTH, D_MODEL, N_EXPERTS), D_MODEL ** -0.5),
        'moe_w_gate': nrm(ks[19], (DEPTH, N_EXPERTS, D_MODEL, EXPERT_FF), D_MODEL ** -0.5),
        'moe_w_up': nrm(ks[20], (DEPTH, N_EXPERTS, D_MODEL, EXPERT_FF), D_MODEL ** -0.5),
        'moe_w_down': nrm(ks[21], (DEPTH, N_EXPERTS, EXPERT_FF, D_MODEL), EXPERT_FF ** -0.5),
    }


def reference(x, c, ctx, c_ctx, w_mod, b_mod, norm_mix, norm_ffn,
              a_w_qkv, a_q_gain, a_k_gain, a_sink, a_w_o,
              b_w_qkv, b_q_gain, b_k_gain, b_rpb, b_w_o,
              moe_router, moe_w_gate, moe_w_up, moe_w_down):
    rope_tabs = axial_rope_tables(x.shape[1])
    xl, xc = x, ctx
    for i in range(DEPTH):
        last = i == DEPTH - 1
        mod_l = jax.nn.silu(c) @ w_mod[i] + b_mod[i]
        mod_c = jax.nn.silu(c_ctx)[None] @ w_mod[i] + b_mod[i]
        sh1_l, sc1_l, g1_l, sh2_l, sc2_l, g2_l = jnp.split(mod_l, 6, axis=-1)
        sh1_c, sc1_c, g1_c, sh2_c, sc2_c, g2_c = jnp.split(mod_c, 6, axis=-1)
        hl = modulate(rms_norm(xl, norm_mix[i]), sh1_l, sc1_l)
        hc = modulate(rms_norm(xc, norm_mix[i]), sh1_c, sc1_c)
        j = i // N_MIXERS
        if i % N_MIXERS == 0:
            yl, yc = window_gqa_sink(hl, hc, a_w_qkv[j], a_q_gain[j], a_k_gain[j], a_sink[j],
                                     a_w_o[j], rope_tabs, not last)
        else:
            yl, yc = neighbourhood_attention(hl, hc, b_w_qkv[j], b_q_gain[j], b_k_gain[j], b_rpb[j],
                                             b_w_o[j], not last)
        xl = xl + g1_l[:, None] * yl
        hl = modulate(rms_norm(xl, norm_ffn[i]), sh2_l, sc2_l)
        xl = xl + g2_l[:, None] * ec_moe(hl, moe_router[i], moe_w_gate[i], moe_w_up[i], moe_w_down[i])
        if not last:
            xc = xc + g1_c[:, None] * yc
            hc = modulate(rms_norm(xc, norm_ffn[i]), sh2_c, sc2_c)
            xc = xc + g2_c[:, None] * ec_moe(hc, moe_router[i], moe_w_gate[i], moe_w_up[i], moe_w_down[i])
    return xl
```

```python
import contextlib
import numpy as np
import concourse.bass as bass
import concourse.mybir as mybir
from concourse.bass_utils import run_bass_kernel_spmd

F32 = mybir.dt.float32
BF16 = mybir.dt.bfloat16
I32 = mybir.dt.int32
ALU = mybir.AluOpType
AF = mybir.ActivationFunctionType
AX = mybir.AxisListType

D = 1024
KC = 8
NE = 16
FF = 2048
CT = 256
EPS = 1e-6
SCALE = 0.125
BIG = float(2 ** 20)
ROWW = 1041
CCAP = 32
NOALIAS = False
DEFER_POST = 2
SKIP_MOE = False
STORE_FG = False
DEBUG = False
NEGM = -30000.0


class Prog:
    ENGS = ("pe", "act", "dve", "pool", "sp")
    NDMASEM = 16

    def __init__(self, nc):
        self.nc = nc
        self.ops = []
        self.last_w = {}
        self.readers = {}
        self.last_eng = {}
        self.dmas = []
        self.emitted = 0
        self.stack = contextlib.ExitStack()
        self.sems = {}
        for e in self.ENGS:
            self.sems[(e, "c")] = self.stack.enter_context(nc.semaphore(f"c_{e}"))
        for e in ("sp", "pool"):
            for k in range(self.NDMASEM):
                self.sems[(e, k)] = self.stack.enter_context(nc.semaphore(f"d_{e}_{k}"))
        self.ms = {e: 0 for e in self.ENGS}
        self.rr = {e: 0 for e in self.ENGS}
        self.dcnt = {}
        self.waited = {e: {} for e in self.ENGS}

    def defer_begin(self):
        self._defer = []

    def defer_end(self):
        d, self._defer = self._defer, None
        return d

    def replay(self, items):
        for it in items:
            if it is not None:
                self.op(*it)

    def mark(self):
        if getattr(self, "_defer", None) is not None:
            self._defer.append(None)

    def op(self, eng, fn, reads=(), writes=(), dma=False, extra=(), force=False):
        if getattr(self, "_defer", None) is not None:
            self._defer.append((eng, fn, tuple(reads), tuple(writes), dma, tuple(extra), force))
            return None
        idx = len(self.ops)
        deps = set(extra)
        for r in reads:
            if r in self.last_w:
                deps.add(self.last_w[r])
        for w in writes:
            if w in self.last_w:
                deps.add(self.last_w[w])
            deps.update(self.readers.get(w, ()))
        for r in reads:
            self.readers.setdefault(r, []).append(idx)
        for w in writes:
            self.last_w[w] = idx
            self.readers[w] = []
        deps.discard(idx)
        self.ops.append(dict(eng=eng, fn=fn, deps=sorted(deps), dma=dma, idx=idx, force=force))
        if dma:
            self.dmas.append(idx)
        else:
            self.last_eng[eng] = idx
        return idx

    def barrier(self):
        deps = list(self.last_eng.values()) + list(self.dmas)
        for e in self.ENGS:
            self.op(e, lambda eng: eng.nop(), extra=deps, force=True)
        self.dmas = []
        self.last_w = {}
        self.readers = {}

    SAME_ENG_SYNC = True

    @classmethod
    def _skip(cls, a, o):
        if a["dma"] or o["dma"]:
            return False
        if a["eng"] != o["eng"]:
            return False
        return a["eng"] == "pe" or not cls.SAME_ENG_SYNC

    def flush(self, final=False):
        nc = self.nc
        ops = self.ops
        batch = ops[self.emitted:]
        base = self.emitted
        needed = set()
        for o in batch:
            if o["force"]:
                needed.add(o["idx"])
            for d in o["deps"]:
                if not self._skip(ops[d], o):
                    if d < base:
                        assert "sem" in ops[d], "cross-phase dependency on unsignalled op"
                    needed.add(d)
        for o in batch:
            if o["dma"]:
                key = (o["eng"], self.rr[o["eng"]] % self.NDMASEM)
                self.rr[o["eng"]] += 1
                prev = self.dcnt.get(key, 0)
                self.dcnt[key] = prev + 16
                o["sem"], o["val"], o["prev"] = key, prev + 16, prev
            elif o["idx"] in needed:
                self.ms[o["eng"]] += 1
                o["sem"], o["val"] = (o["eng"], "c"), self.ms[o["eng"]]
        self.emitted = len(ops)
        sems = self.sems
        engobj = {"pe": "tensor", "act": "scalar", "dve": "vector", "pool": "gpsimd", "sp": "sync"}
        with nc.Block() as block:
            for e in self.ENGS:
                myops = [o for o in batch if o["eng"] == e]

                def body(eng, myops=myops, e=e):
                    waited = self.waited[e]
                    for o in myops:
                        need = {}
                        for d in o["deps"]:
                            a = ops[d]
                            if self._skip(a, o):
                                continue
                            if a["val"] > need.get(a["sem"], 0):
                                need[a["sem"]] = a["val"]
                        if o["dma"] and o["prev"] > 0:
                            need[o["sem"]] = max(need.get(o["sem"], 0), o["prev"])
                        for k, v in need.items():
                            if waited.get(k, 0) < v:
                                waited[k] = v
                                eng.wait_ge(sems[k], v)
                        ins = o["fn"](eng)
                        if "sem" in o:
                            ins.then_inc(sems[o["sem"]], 16 if o["dma"] else 1)
                    if final:
                        for (ee, k), v in self.dcnt.items():
                            if ee == e and waited.get((ee, k), 0) < v:
                                waited[(ee, k)] = v
                                eng.wait_ge(sems[(ee, k)], v)
                getattr(block, engobj[e])(body)
        if final:
            self.stack.close()


def build(T, n_layers=2):
    NT = T // 128
    NCT = CT // 128
    cap = T // 8
    XR = cap + 128
    nc = bass.Bass("TRN2", target_bir_lowering=False)
    P = Prog(nc)

    def din(name, shape, dt=F32):
        return nc.dram_tensor(name, shape, dt, kind="ExternalInput").ap()

    x_in = din("x", [T, D])
    ctx_in = din("ctx", [CT, D])
    cvec = din("cvec", [2, 128, KC])
    w_mod = din("w_mod", [2, D, 6 * D])
    b_mod = din("b_mod", [2, 6 * D])
    norm_mix = din("norm_mix", [2, D])
    norm_ffn = din("norm_ffn", [2, D])
    wqkv_d = [din("a_w_qkv", [D, 1536]), din("b_w_qkv", [D, 3072])]
    qg_d = [din("a_q_gain", [64]), din("b_q_gain", [64])]
    kg_d = [din("a_k_gain", [64]), din("b_k_gain", [64])]
    sink_d = din("a_sink", [16])
    wo_d = [din("a_w_o", [D, D]), din("b_w_o", [D, D])]
    bias_tab = din("bias_tab", [5, 128, 16 * 5 * 128])
    mask0_d = din("mask0", [128, 384])
    ropeC = din("ropeC", [T, 64])
    ropeS = din("ropeS", [T, 64])
    router_d = din("moe_router", [2, D, NE])
    wg_d = din("moe_w_gate", [2, NE, D, FF])
    wu_d = din("moe_w_up", [2, NE, D, FF])
    wd_d = din("moe_w_down", [2, NE, FF, D])
    out = nc.dram_tensor("out", [T, D], F32, kind="ExternalOutput").ap()
    xc_d = nc.dram_tensor("xc_d", [CT, D], F32, kind=("ExternalOutput" if DEBUG else "Internal")).ap()
    h2_d = nc.dram_tensor("h2_d", [T + CT, ROWW], F32).ap()
    aff_d = nc.dram_tensor("aff_d", [T + CT, NE], F32).ap()
    xg_d = [nc.dram_tensor(f"xg_d{e}", [XR, ROWW], F32).ap() for e in range(NE)]
    eb_d = nc.dram_tensor("eb_d", [5, 128, 16 * 5 * 128], BF16).ap()

    uid = [0]

    def sbuf(es, name, shape, dt):
        uid[0] += 1
        return es.enter_context(nc.sbuf_tensor(f"{name}_{uid[0]}", shape, dt))

    def psum(es, name, shape, dt):
        return es.enter_context(nc.psum_tensor(name, shape, dt))

    def MM(o, l, r_, st, sp_, R, W):
        P.op("pe", lambda e: e.matmul(o, lhsT=l, rhs=r_, start=st, stop=sp_), R, W)

    def TR(o, i, idn, R, W):
        P.op("pe", lambda e: e.transpose(o, in_=i, identity=idn), R, W)

    def ACT(o, i, func, R, W, scale=1.0, bias=0.0, accum=None):
        if accum is None:
            P.op("act", lambda e: e.activation(out=o, in_=i, func=func, bias=bias, scale=scale), R, W)
        else:
            P.op("act", lambda e: e.activation(out=o, in_=i, func=func, bias=bias, scale=scale, accum_out=accum), R, W)

    def TT(eng, o, a, b, op, R, W):
        P.op(eng, lambda e: e.tensor_tensor(out=o, in0=a, in1=b, op=op), R, W)

    def TS(eng, o, a, s1, s2, op0, op1, R, W, accum=None):
        if accum is None:
            P.op(eng, lambda e: e.tensor_scalar(out=o, in0=a, scalar1=s1, scalar2=s2, op0=op0, op1=op1), R, W)
        else:
            P.op(eng, lambda e: e.tensor_scalar(out=o, in0=a, scalar1=s1, scalar2=s2, op0=op0, op1=op1, accum_out=accum), R, W)

    def STT(eng, o, a, s, b, op0, op1, R, W):
        P.op(eng, lambda e: e.scalar_tensor_tensor(out=o, in0=a, scalar=s, in1=b, op0=op0, op1=op1), R, W)

    def CP(eng, o, i, R, W):
        if eng == "act":
            P.op("act", lambda e: e.copy(out=o, in_=i), R, W)
        else:
            P.op(eng, lambda e: e.tensor_copy(out=o, in_=i), R, W)

    def RECIP(o, i, R, W):
        P.op("dve", lambda e: e.reciprocal(out=o, in_=i), R, W)

    def DMA(eng, o, i, R, W):
        P.op(eng, lambda e: e.dma_start(out=o, in_=i), R, W, dma=True)

    bcregs = {}

    def bc(e, val):
        if val not in bcregs:
            r = e.alloc_register(f"bc{val}")
            e.reg_mov(r, val)
            bcregs[val] = r
        return bcregs[val]

    def MEMSET(eng, o, v, W):
        P.op(eng, lambda e: e.memset(o, v), (), W)

    with contextlib.ExitStack() as top:
        ident = sbuf(top, "ident", [128, 128], BF16)
        identf = sbuf(top, "identf", [128, 128], F32)
        modrep = sbuf(top, "modrep", [128, 6, D], F32)
        psT = psum(top, "psT", [128, 8, 128], BF16)
        psW = [psum(top, f"psW{i}", [128, 512], F32) for i in range(2)]
        psS = [psum(top, f"psS{i}", [128, 512], F32) for i in range(2)]
        psO = [psum(top, f"psO{i}", [128, 512], F32) for i in range(3)]

        MEMSET("pool", ident[:], 0.0, ["ident"])
        P.op("pool", lambda e: e.affine_select(out=ident[:], in_=ident[:], pattern=[[-1, 128]],
                                               compare_op=ALU.not_equal, fill=1.0, base=0, channel_multiplier=1),
             ["ident"], ["ident"])
        MEMSET("pool", identf[:], 0.0, ["identf"])
        P.op("pool", lambda e: e.affine_select(out=identf[:], in_=identf[:], pattern=[[-1, 128]],
                                               compare_op=ALU.not_equal, fill=1.0, base=0, channel_multiplier=1),
             ["identf"], ["identf"])

        if n_layers > 1:
            with contextlib.ExitStack() as es:
                bt = [sbuf(es, f"bt{i}", [128, 2048], F32) for i in range(2)]
                bo = [sbuf(es, f"bo{i}", [128, 2048], BF16) for i in range(2)]
                n = 0
                for v in range(5):
                    for c0 in range(0, 10240, 2048):
                        i = n % 2
                        n += 1
                        DMA("sp", bt[i][:], bias_tab[v, :, c0:c0 + 2048], [], [f"bt{i}"])
                        ACT(bo[i][:], bt[i][:], AF.Exp, [f"bt{i}"], [f"bo{i}"])
                        DMA("sp", eb_d[v, :, c0:c0 + 2048], bo[i][:], [f"bo{i}"], ["eb_d"])
                P.barrier()
                P.flush()

        def modulation2(l, modc, nvc):
            with contextlib.ExitStack() as es:
                cv = sbuf(es, "cv", [128, 2, KC], F32)
                sc = sbuf(es, "sc", [128, 2, KC], F32)
                lrep = sbuf(es, "lrep", [128, 2, KC, 128], F32)
                wm = [sbuf(es, f"wm{i}", [128, KC, 512], F32) for i in range(2)]
                brep = sbuf(es, "brep", [128, 6 * D], F32)
                nrep = sbuf(es, "nrep", [128, 2, D], F32)
                raws = [sbuf(es, f"modraw{w}", [128, 6, D], F32) for w in range(2)]
                for w in range(2):
                    DMA("sp", cv[:, w, :], cvec[w], [], [f"cv{w}"])
                DMA("sp", brep[:], b_mod[l].partition_broadcast(128), [], ["brep"])
                DMA("sp", nrep[:, 0, :], norm_mix[l].partition_broadcast(128), [], ["nrep0"])
                DMA("sp", nrep[:, 1, :], norm_ffn[l].partition_broadcast(128), [], ["nrep1"])
                for w in range(2):
                    ACT(sc[:, w, :], cv[:, w, :], AF.Silu, [f"cv{w}"], [f"sc{w}"])
                    for k in range(KC):
                        CP("dve", lrep[:, w, k, :], sc[:, w, k:k + 1].to_broadcast([128, 128]), [f"sc{w}"], [f"lrep{w}_{k}"])
                wsrc = w_mod[l].rearrange("(k p) n -> p k n", p=128)
                for j in range(12):
                    i = j % 2
                    DMA("sp", wm[i][:], wsrc[:, :, j * 512:(j + 1) * 512], [], [f"wm{i}"])
                    for w in range(2):
                        ps_, pk_ = (psW[i], f"psW{i}") if w == 0 else (psS[i], f"psS{i}")
                        for k in range(KC):
                            MM(ps_[:], lrep[:, w, k, :], wm[i][:, k, :], k == 0, k == KC - 1,
                               [f"lrep{w}_{k}", f"wm{i}"], [pk_])
                        mflat = raws[w][:].rearrange("p a d -> p (a d)")
                        TT("dve", mflat[:, j * 512:(j + 1) * 512], ps_[:], brep[:, j * 512:(j + 1) * 512], ALU.add,
                           [pk_, "brep"], [f"raw{w}_{j // 2}"])
                for w in range(2):
                    STT("dve", raws[w][:, 1, :], raws[w][:, 1, :], 1.0, nrep[:, 0, :], ALU.add, ALU.mult,
                        [f"raw{w}_1", "nrep0"], [f"raw{w}_1"])
                    STT("dve", raws[w][:, 4, :], raws[w][:, 4, :], 1.0, nrep[:, 1, :], ALU.add, ALU.mult,
                        [f"raw{w}_4", "nrep1"], [f"raw{w}_4"])
                for v in range(6):
                    CP("pool" if v % 2 else "dve", modrep[:, v, :], raws[0][:, v, :], [f"raw0_{v}"], [f"mod{v}"])
                for v in range(nvc):
                    CP("dve" if v % 2 else "pool", modc[:, v, :], raws[1][:, v, :], [f"raw1_{v}"], [f"modc{v}"])
                P.barrier()
                P.flush()

        MODKEYS = [f"mod{i}" for i in range(6)]

        def attention(l, modc):
            nkv = 4 if l == 0 else 16
            NQKV = (16 + 2 * nkv) * 64
            koff = 1024
            voff = 1024 + nkv * 64
            W = 1 if l == 0 else 2
            NS = 2 * W + 2
            NHS = W + 2
            nkp = 4 if l == 0 else 8
            rope = (l == 0)
            last = (l == n_layers - 1)
            with contextlib.ExitStack() as es:
                wqkv = sbuf(es, "wqkv", [128, KC, NQKV], BF16)
                wo = sbuf(es, "wo", [128, KC, D], BF16)
                wr = sbuf(es, "wr", [128, KC, NE], BF16)
                qg = sbuf(es, "qg", [128, 64], F32)
                kg = sbuf(es, "kg", [128, 64], F32)
                sinke = sbuf(es, "sinke", [128, 16], F32)
                mhalf = sbuf(es, "mhalf", [128, 16], F32)
                KT = sbuf(es, "KT", [128, NS + NCT, nkp, 128], BF16)
                VA = sbuf(es, "VA", [128, NS + NCT, nkv, 65], BF16)
                hT = sbuf(es, "hT", [128, D], BF16)
                xt = sbuf(es, "xt", [128, D], F32)
                Sa = sbuf(es, "Sa", [128, D], F32)
                Sc = sbuf(es, "Sc", [128, D], F32)
                hb = sbuf(es, "hb", [128, D], BF16)
                qn = sbuf(es, "qn", [128, D], BF16)
                kn = sbuf(es, "kn", [128, D], BF16)
                QTr = sbuf(es, "QTr", [128, NHS + (NCT if l == 0 else 0), 8, 128], BF16)
                Pb = [sbuf(es, f"Pb{i}", [128, 896], BF16) for i in range(2)]
                Osb = sbuf(es, "Osb", [128, D], BF16)
                OT = sbuf(es, "OT", [128, D], BF16)
                h2a = sbuf(es, "h2a", [128, ROWW], F32)
                tidi = sbuf(es, "tidi", [128, 1], I32)
                h2T = sbuf(es, "h2T", [128, D], BF16)
                afft = sbuf(es, "afft", [128, NE], F32)
                if l == 0:
                    mtab = sbuf(es, "mtab", [128, 384], BF16)
                else:
                    mtab = sbuf(es, "mtab", [128, 16, 640], BF16)

                def scratch(tag, full):
                    s = dict(tag=tag)
                    s["sm"] = sbuf(es, "sm" + tag, [128, 128], F32)
                    s["nsq"] = sbuf(es, "nsq" + tag, [128, 512], F32)
                    s["nt1"] = sbuf(es, "nt1" + tag, [128, 512], F32)
                    if rope:
                        for nm in ("nt2", "nt3", "nt4"):
                            s[nm] = sbuf(es, nm + tag, [128, 512], F32)
                        s["rC"] = sbuf(es, "rC" + tag, [128, 64], F32)
                        s["rS"] = sbuf(es, "rS" + tag, [128, 64], F32)
                    return s

                scA = dict(tag="A")
                scA["sm"] = sbuf(es, "smA", [128, 128], F32)
                scA["nsq"] = sbuf(es, "nsqA", [128, 512], F32)
                scA["nt1"] = sbuf(es, "nt1A", [128, 512], F32)
                scA["Sa"] = Sa
                scA["hb"] = hb
                scP = dict(tag="P")
                scP["sm"] = sbuf(es, "smP", [128, 128], F32)
                scP["nt1"] = sbuf(es, "nt1P", [128, 512], F32)
                if rope:
                    for s_ in (scA, scP):
                        for nm in ("nt2", "nt3", "nt4"):
                            s_[nm] = sbuf(es, nm + s_["tag"], [128, 512], F32)
                        s_["rC"] = sbuf(es, "rC" + s_["tag"], [128, 64], F32)
                        s_["rS"] = sbuf(es, "rS" + s_["tag"], [128, 64], F32)
                if l == 0 or NOALIAS:
                    scP["nsq"] = sbuf(es, "nsqP", [128, 512], F32)
                    scP["Sa"] = sbuf(es, "SaP", [128, D], F32)
                    scP["hb"] = sbuf(es, "hbP", [128, D], BF16)
                else:
                    scP["Sa"] = modc[:, 0, :]
                    scP["hb"] = modc[:, 1, 0:512].bitcast(BF16)
                    scP["nsq"] = modc[:, 1, 512:1024]

                WQK = [f"wqkv{k}_{n0}" for k in range(KC) for n0 in range(0, NQKV, 1024)]
                for k in range(KC):
                    for n0 in range(0, NQKV, 1024):
                        n1 = min(NQKV, n0 + 1024)
                        DMA("pool", wqkv[:, k, n0:n1], wqkv_d[l][k * 128:(k + 1) * 128, n0:n1], [], [f"wqkv{k}_{n0}"])
                    DMA("pool", wo[:, k, :], wo_d[l][k * 128:(k + 1) * 128, :], [], [f"wo{k}"])
                DMA("pool", wr[:], router_d[l].rearrange("(k p) e -> p k e", p=128), [], ["wr"])
                DMA("sp", qg[:], qg_d[l].partition_broadcast(128), [], ["qg"])
                DMA("sp", kg[:], kg_d[l].partition_broadcast(128), [], ["kg"])
                MEMSET("pool", mhalf[:], -0.5, ["mhalf"])
                if l == 0:
                    DMA("sp", sinke[:], sink_d.partition_broadcast(128), [], ["sinke"])
                    ACT(sinke[:], sinke[:], AF.Exp, ["sinke"], ["sinke"])
                    DMA("pool", mtab[:], mask0_d, [], ["mtab"])
                else:
                    MEMSET("pool", sinke[:], 0.0, ["sinke"])
                MEMSET("pool", VA[:, :, :, 64:65], 1.0, ["VAones"])

                def rstd_of(sc_, src_ap, dst_ap, n, inv, kin, kout):
                    t = sc_["tag"]
                    TS("pool", dst_ap, src_ap, inv, EPS, ALU.mult, ALU.add, [kin], [kout + "_t"])
                    TT("pool", dst_ap, dst_ap, mhalf[:, 0:n], ALU.pow, [kout + "_t", "mhalf"], [kout])

                def norm_seg(sc_, src, nh, gain, keyg, use_rope, dst, R, Wk):
                    t = sc_["tag"]
                    sm = sc_["sm"]
                    nsq, nt1 = sc_["nsq"], sc_["nt1"]
                    n = nh * 64
                    s3 = src.rearrange("p (h d) -> p h d", d=64)
                    ACT(nsq[:, 0:n], src, AF.Square, R, ["nsq" + t])
                    P.op("dve", lambda e: e.reduce_sum(out=sm[:, 0:nh], in_=nsq[:, 0:n].rearrange("p (h d) -> p h d", d=64), axis=AX.X),
                         ["nsq" + t], ["ssq" + t])
                    rstd_of(sc_, sm[:, 0:nh], sm[:, 32:32 + nh], nh, 1.0 / 64, "ssq" + t, "rs" + t)
                    t1 = nt1[:, 0:n].rearrange("p (h d) -> p h d", d=64)
                    TT("dve", t1, s3, gain[:].unsqueeze(1).to_broadcast([128, nh, 64]), ALU.mult, R + [keyg], ["nt1" + t])
                    rsb = sm[:, 32:32 + nh].unsqueeze(2).to_broadcast([128, nh, 64])
                    if not use_rope:
                        TT("dve", dst, t1, rsb, ALU.mult, ["nt1" + t, "rs" + t], Wk)
                        return
                    nt2, nt3, nt4, rC, rS = sc_["nt2"], sc_["nt3"], sc_["nt4"], sc_["rC"], sc_["rS"]
                    t2 = nt2[:, 0:n].rearrange("p (h d) -> p h d", d=64)
                    TT("dve", t2, t1, rsb, ALU.mult, ["nt1" + t, "rs" + t], ["nt2" + t])
                    t3 = nt3[:, 0:n].rearrange("p (h d) -> p h d", d=64)
                    TT("pool", t3, t2, rC[:].unsqueeze(1).to_broadcast([128, nh, 64]), ALU.mult, ["nt2" + t, "rC" + t], ["nt3" + t])
                    t2v = nt2[:, 0:n].rearrange("p (h a g d) -> p h a g d", a=2, g=2, d=16)
                    t4v = nt4[:, 0:n].rearrange("p (h a g d) -> p h a g d", a=2, g=2, d=16)
                    rSv = rS[:].rearrange("p (a g d) -> p a g d", a=2, g=2)
                    TT("dve", t4v[:, :, :, 0, :], t2v[:, :, :, 1, :],
                       rSv[:, :, 0, :].unsqueeze(1).to_broadcast([128, nh, 2, 16]), ALU.mult, ["nt2" + t, "rS" + t], ["nt4a" + t])
                    TT("dve", t4v[:, :, :, 1, :], t2v[:, :, :, 0, :],
                       rSv[:, :, 1, :].unsqueeze(1).to_broadcast([128, nh, 2, 16]), ALU.mult, ["nt2" + t, "rS" + t], ["nt4b" + t])
                    t4 = nt4[:, 0:n].rearrange("p (h d) -> p h d", d=64)
                    TT("dve", dst, t3, t4, ALU.add, ["nt3" + t, "nt4a" + t, "nt4b" + t], Wk)

                def prenorm(sc_, src_t, keysrc, gi, dst_bf, keydst, mod, mk, dst32=None, key32=None):
                    t = sc_["tag"]
                    sm = sc_["sm"]
                    Sx = sc_["Sa"]
                    ACT(Sx[:], src_t, AF.Square, [keysrc], ["Sa" + t, "ss" + t], accum=sm[:, 64:65])
                    rstd_of(sc_, sm[:, 64:65], sm[:, 66:67], 1, 1.0 / D, "ss" + t, "r" + t)
                    STT("dve", Sx[:], src_t, sm[:, 66:67], mod[:, gi + 1, :], ALU.mult, ALU.mult,
                        [keysrc, "r" + t, f"{mk}{gi + 1}"], ["Sa" + t])
                    TT("dve", dst_bf, Sx[:], mod[:, gi, :], ALU.add, ["Sa" + t, f"{mk}{gi}"], [keydst])
                    if dst32 is not None:
                        TT("pool", dst32, Sx[:], mod[:, gi, :], ALU.add, ["Sa" + t, f"{mk}{gi}"], [key32])

                def tile_info(kind, i):
                    if kind == "l":
                        src = (x_in if l == 0 else out)[i * 128:(i + 1) * 128, :]
                        return src, f"xl{i}", i % NS, i % NHS, out[i * 128:(i + 1) * 128, :]
                    src = (ctx_in if l == 0 else xc_d)[i * 128:(i + 1) * 128, :]
                    return src, f"xc{i}", NS + i, NHS + (i if l == 0 else 0), xc_d[i * 128:(i + 1) * 128, :]

                def project(kind, i):
                    src, dkey, slot, hs, _ = tile_info(kind, i)
                    use_rope = rope and kind == "l"
                    sc_ = scP if kind == "l" else scA
                    t = sc_["tag"]
                    hbx = sc_["hb"]
                    mod, mk = (modrep, "mod") if kind == "l" else (modc, "modc")
                    DMA("sp", xt[:], src, [dkey], ["xt"])
                    if use_rope:
                        DMA("sp", sc_["rC"][:], ropeC[i * 128:(i + 1) * 128, :], [], ["rC" + t])
                        DMA("sp", sc_["rS"][:], ropeS[i * 128:(i + 1) * 128, :], [], ["rS" + t])
                    prenorm(sc_, xt[:], "xt", 0, hbx[:], "hb" + t, mod, mk)
                    for k in range(KC):
                        TR(psT[:, k, :], hbx[:, k * 128:(k + 1) * 128], ident[:], ["hb" + t, "ident"], ["psT"])
                    CP("act", hT[:], psT[:].rearrange("p k t -> p (k t)"), ["psT"], ["hT"])
                    P.mark()
                    hTv = hT[:].rearrange("p (k t) -> p k t", k=KC)
                    if kind == "l" or l == 0:
                        for j in range(2):
                            pw = psW[j]
                            for k in range(KC):
                                MM(pw[:], hTv[:, k, :], wqkv[:, k, j * 512:(j + 1) * 512], k == 0, k == KC - 1,
                                   ["hT"] + WQK, [f"psW{j}"])
                            norm_seg(sc_, pw[:], 8, qg, "qg", use_rope,
                                     qn[:, j * 512:(j + 1) * 512].rearrange("p (h d) -> p h d", d=64), [f"psW{j}"], [f"qn{j}"])
                            P.mark()
                        for j in range(8):
                            TR(psT[:, j, :], qn[:, j * 128:(j + 1) * 128], ident[:], ["qn0", "qn1", "ident"], ["psT"])
                        CP("act", QTr[:, hs, :, :], psT[:], ["psT"], [f"QT{hs}"])
                        P.mark()
                    c0 = koff
                    ci = 0
                    while c0 < NQKV:
                        c1 = min(NQKV, c0 + 512)
                        pw = psW[ci % 2]
                        pk = f"psW{ci % 2}"
                        ci += 1
                        for k in range(KC):
                            MM(pw[:, 0:c1 - c0], hTv[:, k, :], wqkv[:, k, c0:c1], k == 0, k == KC - 1,
                               ["hT"] + WQK, [pk])
                        a0, a1 = max(c0, koff), min(c1, voff)
                        if a1 > a0:
                            nh = (a1 - a0) // 64
                            h0 = (a0 - koff) // 64
                            if l == 0:
                                dst = kn[:, 0:512].rearrange("p (h a d) -> p h a d", a=2, d=64)[:, :, 0, :]
                            else:
                                dst = kn[:, h0 * 64:(h0 + nh) * 64].rearrange("p (h d) -> p h d", d=64)
                            norm_seg(sc_, pw[:, a0 - c0:a1 - c0], nh, kg, "kg", use_rope, dst, [pk], [f"kn{h0 // 8}"])
                        a0, a1 = max(c0, voff), min(c1, NQKV)
                        if a1 > a0:
                            nh = (a1 - a0) // 64
                            h0 = (a0 - voff) // 64
                            CP("act", VA[:, slot, h0:h0 + nh, 0:64],
                               pw[:, a0 - c0:a1 - c0].rearrange("p (h d) -> p h d", d=64), [pk], [f"VA{slot}_{h0 // 8}"])
                        c0 = c1
                        P.mark()
                    if l == 0:
                        knv = kn[:, 0:512].rearrange("p (h a d) -> p h a d", a=2, d=64)
                        CP("pool", knv[:, :, 1, :], knv[:, :, 0, :], ["kn0"], ["kn0b"])
                        for j in range(4):
                            TR(psT[:, j, :], kn[:, j * 128:(j + 1) * 128], ident[:], ["kn0", "kn0b", "ident"], ["psT"])
                    else:
                        for j in range(8):
                            TR(psT[:, j, :], kn[:, j * 128:(j + 1) * 128], ident[:], ["kn0", "kn1", "ident"], ["psT"])
                    CP("act", KT[:, slot, :, :], psT[:, 0:nkp, :], ["psT"], [f"KT{slot}"])

                cur = dict(var=-1)

                def heads(kind, i, pending):
                    src, dkey, slot, hs, dstrows = tile_info(kind, i)
                    sm = scA["sm"]
                    QT = QTr[:, hs, :, :]
                    qtk = f"QT{hs}"
                    chunks = []
                    if kind == "l":
                        dlo = max(-W, -i)
                        dhi = min(W, NT - 1 - i)
                        if l == 1:
                            if i == 0:
                                dlo, dhi = 0, min(3, NT - 1)
                            elif i == NT - 1:
                                dlo, dhi = max(-3, -i), 0
                        for dl in range(dlo, dhi + 1):
                            chunks.append((i + dl) % NS)
                        nl = len(chunks)
                    else:
                        nl = 0
                        dlo = 0
                    chunks += [NS + j for j in range(NCT)]
                    nch = len(chunks)
                    nA = min(4, nch)
                    nB = nch - nA
                    if kind == "l" and l == 1:
                        v = 0 if i == 0 else 1 if i == 1 else 3 if i == NT - 2 else 4 if i == NT - 1 else 2
                        if cur["var"] != v:
                            cur["var"] = v
                            DMA("sp", mtab[:].rearrange("p h c -> p (h c)"), eb_d[v], ["eb_d"], ["mtab"])

                    def st_phase(h):
                        rows = slice(64 * (h % 2), 64 * (h % 2) + 64)
                        kidx = h // 2 if l == 1 else h // 4
                        hp = h % 2
                        pb = Pb[hp]
                        for c, sl in enumerate(chunks):
                            g = c // 4
                            MM(psS[g][:, (c % 4) * 128:(c % 4 + 1) * 128], KT[rows, sl, kidx, :], QT[rows, h // 2, :],
                               True, True, [f"KT{sl}", qtk], [f"psS{g}"])
                        ACT(pb[:, 0:nA * 128], psS[0][:, 0:nA * 128], AF.Exp, ["psS0"], [f"Pb{hp}a"], scale=SCALE)
                        if nB:
                            ACT(pb[:, 512:512 + nB * 128], psS[1][:, 0:nB * 128], AF.Exp, ["psS1"], [f"Pb{hp}b"], scale=SCALE)
                        if nl:
                            if l == 0:
                                mt = mtab[:, (dlo + W) * 128:(dlo + W + nl) * 128]
                            else:
                                mt = mtab[:, h, 0:nl * 128]
                            TT("dve", pb[:, 0:nl * 128], pb[:, 0:nl * 128], mt, ALU.mult,
                               [f"Pb{hp}a", f"Pb{hp}b", "mtab"], [f"Pb{hp}a", f"Pb{hp}b"])

                    def pv_phase(h):
                        kvh = h if l == 1 else h // 4
                        hp = h % 2
                        pb = Pb[hp]
                        og = h // 6
                        hh = h % 6
                        for c, sl in enumerate(chunks):
                            MM(psO[og][:, hh * 65:(hh + 1) * 65], pb[:, c * 128:(c + 1) * 128], VA[:, sl, kvh, :],
                               c == 0, c == nch - 1,
                               [f"Pb{hp}a", f"Pb{hp}b", f"VA{sl}_{kvh // 8}", "VAones"], [f"psO{og}"])

                    npend = len(pending)
                    per = (npend + 14) // 15 if npend else 0
                    pi = 0
                    st_phase(0)
                    for h in range(16):
                        if h + 1 < 16:
                            st_phase(h + 1)
                        pv_phase(h)
                        if pi < npend:
                            P.replay(pending[pi:pi + per])
                            pi += per
                    if pi < npend:
                        P.replay(pending[pi:])
                    for og in range(3):
                        h0 = og * 6
                        ng = min(6, 16 - h0)
                        pv = psO[og][:, 0:ng * 65].rearrange("p (h d) -> p h d", d=65)
                        TT("dve", sm[:, 80:80 + ng], pv[:, :, 64], sinke[:, h0:h0 + ng], ALU.add,
                           [f"psO{og}", "sinke"], ["sm_den"])
                        RECIP(sm[:, 96:96 + ng], sm[:, 80:80 + ng], ["sm_den"], ["sm_rden"])
                        TT("dve", Osb[:, h0 * 64:(h0 + ng) * 64].rearrange("p (h d) -> p h d", d=64), pv[:, :, 0:64],
                           sm[:, 96:96 + ng].unsqueeze(2).to_broadcast([128, ng, 64]), ALU.mult,
                           [f"psO{og}", "sm_rden"], ["Osb"])

                def post(kind, i, part=7):
                    src, dkey, slot, hs, dstrows = tile_info(kind, i)
                    if part & 1:
                        post_a(kind, i, 1)
                    if part & 4:
                        post_a(kind, i, 2)
                    if part & 2:
                        post_b(kind, i)

                def post_a(kind, i, sub):
                    src, dkey, slot, hs, dstrows = tile_info(kind, i)
                    mod, mk = (modrep, "mod") if kind == "l" else (modc, "modc")
                    if sub == 1:
                        DMA("sp", Sc[:], src, [dkey], ["Sc"])
                        for k in range(KC):
                            TR(psT[:, k, :], Osb[:, k * 128:(k + 1) * 128], ident[:], ["Osb", "ident"], ["psT"])
                        CP("act", OT[:], psT[:].rearrange("p k t -> p (k t)"), ["psT"], ["OT"])
                        P.mark()
                        return
                    OTv = OT[:].rearrange("p (k t) -> p k t", k=KC)
                    for j in range(2):
                        for k in range(KC):
                            MM(psW[j][:], OTv[:, k, :], wo[:, k, j * 512:(j + 1) * 512], k == 0, k == KC - 1,
                               ["OT", f"wo{k}"], [f"psW{j}"])
                        TT("dve", Sa[:, j * 512:(j + 1) * 512], psW[j][:], mod[:, 2, j * 512:(j + 1) * 512], ALU.mult,
                           [f"psW{j}", f"{mk}2"], ["SaA"])
                    TT("dve", Sc[:], Sc[:], Sa[:], ALU.add, ["Sc", "SaA"], ["Sc"])
                    P.mark()
                    if not (last and kind == "c") and not (STORE_FG and getattr(P, "_defer", None) is not None):
                        DMA("sp", dstrows, Sc[:], ["Sc"], [dkey])

                def post_store(kind, i):
                    src, dkey, slot, hs, dstrows = tile_info(kind, i)
                    DMA("sp", dstrows, Sc[:], ["Sc"], [dkey])

                def post_b(kind, i):
                    src, dkey, slot, hs, dstrows = tile_info(kind, i)
                    do_moe = (kind == "l") or (l == 0)
                    mod, mk = (modrep, "mod") if kind == "l" else (modc, "modc")
                    sm = scA["sm"]
                    if not do_moe:
                        return
                    prenorm(scA, Sc[:], "Sc", 3, hb[:], "hbA", mod, mk, h2a[:, 0:D], "h2a")
                    for k in range(KC):
                        TR(psT[:, k, :], hb[:, k * 128:(k + 1) * 128], ident[:], ["hbA", "ident"], ["psT"])
                    CP("act", h2T[:], psT[:].rearrange("p k t -> p (k t)"), ["psT"], ["h2T"])
                    P.mark()
                    h2Tv = h2T[:].rearrange("p (k t) -> p k t", k=KC)
                    for k in range(KC):
                        MM(psW[0][:, 0:NE], h2Tv[:, k, :], wr[:, k, :], k == 0, k == KC - 1, ["h2T", "wr"], ["psW0"])
                    ACT(sm[:, 104:120], psW[0][:, 0:NE], AF.Exp, ["psW0"], ["sm_ex", "sm_es"], accum=sm[:, 120:121])
                    RECIP(sm[:, 121:122], sm[:, 120:121], ["sm_es"], ["sm_er"])
                    TS("dve", afft[:], sm[:, 104:120], sm[:, 121:122], None, ALU.mult, ALU.bypass, ["sm_ex", "sm_er"], ["afft"])
                    CP("dve", h2a[:, 1025:1041], afft[:], ["afft"], ["h2a_aff"])
                    tokbase = i * 128
                    P.op("pool", lambda e: e.iota(tidi[:], pattern=[[0, 1]], base=tokbase, channel_multiplier=1), [], ["tidi"])
                    CP("dve", h2a[:, 1024:1025], tidi[:], ["tidi"], ["h2a_id"])
                    r0 = i * 128 if kind == "l" else T + i * 128
                    DMA("sp", h2_d[r0:r0 + 128, :], h2a[:], ["h2a", "h2a_aff", "h2a_id"], ["h2_d"])
                    DMA("sp", aff_d[r0:r0 + 128, :], afft[:], ["afft"], ["aff_d"])

                def groups(lst):
                    gs, curg = [], []
                    for it in lst:
                        if it is None:
                            if curg:
                                gs.append(curg)
                            curg = []
                        else:
                            curg.append(it)
                    if curg:
                        gs.append(curg)
                    return gs

                def merge(a, b):
                    ga, gb = groups(a), groups(b)
                    out_, ia, ib = [], 0, 0
                    na, nb_ = len(ga), len(gb)
                    while ia < na or ib < nb_:
                        if ib >= nb_ or (ia < na and ia * nb_ <= ib * na):
                            out_ += ga[ia]
                            ia += 1
                        else:
                            out_ += gb[ib]
                            ib += 1
                    return out_

                for j in range(NCT):
                    project("c", j)
                if l == 0:
                    for j in range(NCT):
                        heads("c", j, [])
                        post("c", j)
                P.barrier()
                for i in range(min(W + 1, NT)):
                    project("l", i)
                for i in range(NT):
                    bgA, bgB = [], []
                    if i + W + 1 < NT:
                        if l == 1 and i == 0:
                            project("l", i + W + 1)
                        else:
                            P.defer_begin()
                            project("l", i + W + 1)
                            bgA = P.defer_end()
                    if DEFER_POST == 2:
                        if i >= 1:
                            P.defer_begin()
                            post("l", i - 1, 2)
                            bgB = P.defer_end()
                        heads("l", i, merge(bgA, bgB))
                        post("l", i, 5)
                        if i == NT - 1:
                            post("l", i, 2)
                        continue
                    if i >= 1 and DEFER_POST:
                        P.defer_begin()
                        post("l", i - 1, DEFER_POST)
                        bgB = P.defer_end()
                    heads("l", i, merge(bgA, bgB))
                    if i >= 1 and (DEFER_POST & 4) and STORE_FG:
                        post_store("l", i - 1)
                    if i >= 1 and DEFER_POST and DEFER_POST != 7:
                        post("l", i - 1, 7 - DEFER_POST)
                    if not DEFER_POST:
                        post("l", i)
                if DEFER_POST and DEFER_POST != 2:
                    post("l", NT - 1)
                P.barrier()
                P.flush()

        def moe(l, modc):
            with_ctx = (l == 0)
            ntl = NT + (NCT if with_ctx else 0)
            with contextlib.ExitStack() as es:
                dsti = sbuf(es, "dsti", [128, NT + NCT, NE], I32)
                g2rep = modrep[:, 5, :]
                with contextlib.ExitStack() as es2:
                    afftm = sbuf(es2, "afftm", [128, NT + NCT, NE], F32)
                    affT = sbuf(es2, "affT", [NE, T + CT], F32)
                    wk = sbuf(es2, "wk", [NE, T + CT], F32)
                    wk2 = sbuf(es2, "wk2", [NE, T + CT], F32)
                    st = sbuf(es2, "st", [NE, 32], F32)
                    DMA("sp", afftm[:, 0:ntl, :], aff_d[0:ntl * 128, :].rearrange("(n p) e -> p n e", p=128), ["aff_d"], ["afftm"])
                    for n in range(ntl):
                        pw = psW[n % 2]
                        TR(pw[0:NE, 0:128], afftm[:, n, :], identf[:], ["afftm", "identf"], [f"psW{n % 2}"])
                        CP("act" if n % 2 else "dve", affT[:, n * 128:(n + 1) * 128], pw[0:NE, 0:128], [f"psW{n % 2}"], [f"affT{n // NT}"])
                    sets = [(0, T, cap, 0, 0)]
                    if with_ctx:
                        sets.append((T, T + CT, CCAP, cap, 1))
                    for (t0, t1, cp_, slot0, si) in sets:
                        b = si * 8
                        lo, hi, mid, cnt, ge, d1 = [st[:, b + j:b + j + 1] for j in range(6)]
                        kk = f"st{si}"
                        MEMSET("dve", lo, 0.0, [kk])
                        MEMSET("dve", hi, 1.0, [kk])
                        MEMSET("dve", mid, 0.5, [kk])
                        for it in range(30):
                            TS("dve", wk[:, t0:t1], affT[:, t0:t1], mid, None, ALU.is_gt, ALU.add, [f"affT{si}", kk], [f"wk{si}", kk], accum=cnt)
                            TS("dve", ge, cnt, cp_ - 0.5, None, ALU.is_gt, ALU.bypass, [kk], [kk])
                            TT("dve", d1, mid, lo, ALU.subtract, [kk], [kk])
                            STT("dve", lo, d1, ge, lo, ALU.mult, ALU.add, [kk], [kk])
                            TT("dve", d1, hi, mid, ALU.subtract, [kk], [kk])
                            STT("dve", hi, d1, ge, mid, ALU.mult, ALU.add, [kk], [kk])
                            STT("dve", mid, lo, 1.0, hi, ALU.mult, ALU.add, [kk], [kk])
                            TS("dve", mid, mid, 0.5, None, ALU.mult, ALU.bypass, [kk], [kk])
                        TS("dve", wk[:, t0:t1], affT[:, t0:t1], lo, None, ALU.is_gt, ALU.bypass, [f"affT{si}", kk], [f"wk{si}"])
                        P.op("dve", lambda e, t0=t0, t1=t1: e.tensor_tensor_scan(out=wk2[:, t0:t1], data0=wk[:, t0:t1], data1=wk[:, t0:t1],
                                                                                 initial=0.0, op0=ALU.add, op1=ALU.max),
                             [f"wk{si}"], [f"wk2{si}"])
                        TS("dve", affT[:, t0:t1], wk2[:, t0:t1], cp_ + 0.5, None, ALU.is_lt, ALU.bypass, [f"wk2{si}"], [f"affT{si}"])
                        TT("dve", wk[:, t0:t1], wk[:, t0:t1], affT[:, t0:t1], ALU.mult, [f"wk{si}", f"affT{si}"], [f"wk{si}"])
                        TS("dve", wk2[:, t0:t1], wk2[:, t0:t1], BIG - 1.0 + slot0, None, ALU.add, ALU.bypass, [f"wk2{si}"], [f"wk2{si}"])
                        STT("dve", wk2[:, t0:t1], wk[:, t0:t1], -BIG, wk2[:, t0:t1], ALU.mult, ALU.add, [f"wk{si}", f"wk2{si}"], [f"wk2{si}"])
                    for n in range(ntl):
                        pw = psW[n % 2]
                        si = n // NT
                        TR(pw[:, 0:NE], wk2[:, n * 128:(n + 1) * 128], identf[0:NE, 0:NE], [f"wk2{si}", "identf"], [f"psW{n % 2}"])
                        CP("dve", dsti[:, n, :], pw[:, 0:NE], [f"psW{n % 2}"], ["dsti"])
                    P.barrier()
                    P.flush()
                with contextlib.ExitStack() as es3:
                    NHR = 4
                    hrow = [sbuf(es3, f"hrow{i}", [128, ROWW], F32) for i in range(NHR)]
                    xgT = sbuf(es3, "xgT", [128, KC, cap + CCAP], BF16)
                    hTt = sbuf(es3, "hTt", [128, 16, cap + CCAP], BF16)
                    wgu = [sbuf(es3, f"wgu{i}", [128, 2, KC, 512], BF16) for i in range(2)]
                    wdn = sbuf(es3, "wdn", [128, 16, D], BF16)
                    xgc = [sbuf(es3, f"xgc{i}", [128, ROWW], F32) for i in range(2)]
                    xgb = [sbuf(es3, f"xgb{i}", [128, D], BF16) for i in range(2)]
                    yv = [sbuf(es3, f"yv{i}", [128, D], F32) for i in range(2)]
                    sil = [sbuf(es3, f"sil{i}", [128, 512], F32) for i in range(2)]
                    tokg = [sbuf(es3, f"tokg{i}", [128, 16], I32) for i in range(2)]
                    gatg = [sbuf(es3, f"gatg{i}", [128, 16], F32) for i in range(2)]
                    NG = 4
                    EPG = NE // NG
                    hcnt = [0]

                    def dispatch_ops(g):
                        P.defer_begin()
                        slots = []

                        def load(n):
                            b_ = hcnt[0] % NHR
                            hcnt[0] += 1
                            slots.append(b_)
                            DMA("sp", hrow[b_][:], h2_d[n * 128:(n + 1) * 128, :], ["h2_d"], [f"hrow{b_}"])

                        for n in range(min(NHR - 1, ntl)):
                            load(n)
                        for n in range(ntl):
                            b_ = slots[n]
                            hr = hrow[b_]
                            for e_ in range(g * EPG, (g + 1) * EPG):
                                P.op("pool", lambda e, hr=hr, n=n, e_=e_: e.indirect_dma_start(
                                    out=xg_d[e_], out_offset=bass.IndirectOffsetOnAxis(ap=dsti[:, n, e_:e_ + 1], axis=0),
                                    in_=hr[:], in_offset=None, bounds_check=bc(e, XR - 1), oob_is_err=False),
                                    [f"hrow{b_}", "dsti"], [f"xg{e_}_{n}"], dma=True)
                            if n + NHR - 1 < ntl:
                                load(n + NHR - 1)
                        return P.defer_end()

                    P.replay(dispatch_ops(0))
                    nlc = cap // 128
                    tokchunks = [(c * 128, 128) for c in range(nlc)]
                    if with_ctx:
                        tokchunks.append((cap, CCAP))
                    ntok = cap + (CCAP if with_ctx else 0)
                    ncols = []
                    c0 = 0
                    while c0 < ntok:
                        ncols.append((c0, min(512, ntok - c0)))
                        c0 += 512
                    nyv = 0
                    unit = 0

                    def load_unit(e_, q):
                        nonlocal unit
                        u = unit % 2
                        unit += 1
                        for k in range(KC):
                            DMA("pool", wgu[u][:, 0, k, :], wg_d[l, e_, k * 128:(k + 1) * 128, q * 512:(q + 1) * 512], [], [f"wgu{u}g{k}"])
                            DMA("pool", wgu[u][:, 1, k, :], wu_d[l, e_, k * 128:(k + 1) * 128, q * 512:(q + 1) * 512], [], [f"wgu{u}u{k}"])
                        return u

                    def load_wd(e_):
                        for f in range(16):
                            DMA("pool", wdn[:, f, :], wd_d[l, e_, f * 128:(f + 1) * 128, :], [], [f"wdn{f}"])

                    def gather_T(e_, only=None):
                        pe_ = e_ % 2
                        for ci, (r0, nr) in enumerate(tokchunks):
                            if only is not None and ci != only:
                                continue
                            xc_ = xgc[ci % 2]
                            DMA("sp", xc_[0:nr, :], xg_d[e_][r0:r0 + nr, :], [f"xg{e_}_{n}" for n in range(ntl)], [f"xgc{ci % 2}"])
                            xb_ = xgb[ci % 2]
                            CP("dve", xb_[0:nr, :], xc_[0:nr, 0:D], [f"xgc{ci % 2}"], [f"xgb{ci % 2}"])
                            for k in range(KC):
                                TR(psT[:, k, 0:nr], xb_[0:nr, k * 128:(k + 1) * 128], ident[0:nr, 0:nr], [f"xgb{ci % 2}", "ident"], ["psT"])
                            CP("act", xgT[:, :, r0:r0 + nr], psT[:, :, 0:nr], ["psT"], ["xgT"])
                            CP("pool", tokg[pe_][0:nr, ci:ci + 1], xc_[0:nr, 1024:1025], [f"xgc{ci % 2}"], [f"tokg{pe_}"])
                            CP("pool", gatg[pe_][0:nr, ci:ci + 1], xc_[0:nr, 1025 + e_:1026 + e_], [f"xgc{ci % 2}"], [f"gatg{pe_}"])

                    git = [0]

                    def gateup(e_, q, u):
                        for fi in range(4):
                            f = q * 4 + fi
                            for (n0, nn) in ncols:
                                s = git[0] % 2
                                git[0] += 1
                                G, U = (psS[0], psS[1]) if s == 0 else (psW[0], psW[1])
                                gk, uk = ("psS0", "psS1") if s == 0 else ("psW0", "psW1")
                                for k in range(KC):
                                    MM(G[:, 0:nn], wgu[u][:, 0, k, fi * 128:(fi + 1) * 128], xgT[:, k, n0:n0 + nn], k == 0, k == KC - 1,
                                       [f"wgu{u}g{k}", "xgT"], [gk])
                                for k in range(KC):
                                    MM(U[:, 0:nn], wgu[u][:, 1, k, fi * 128:(fi + 1) * 128], xgT[:, k, n0:n0 + nn], k == 0, k == KC - 1,
                                       [f"wgu{u}u{k}", "xgT"], [uk])
                                s_ = sil[s]
                                sk = f"sil{s}"
                                ACT(s_[:, 0:nn], G[:, 0:nn], AF.Silu, [gk], [sk])
                                TT("dve", hTt[:, f, n0:n0 + nn], s_[:, 0:nn], U[:, 0:nn], ALU.mult, [sk, uk], [f"hTt{f}"])

                    def down(e_, only=None):
                        nonlocal nyv
                        pe_ = e_ % 2
                        for ci, (r0, nr) in enumerate(tokchunks):
                            if only is not None and ci != only:
                                continue
                            y_ = yv[nyv % 2]
                            yk = f"yv{nyv % 2}"
                            nyv += 1
                            is_ctx = (r0 >= cap)
                            gsrc = modc[:, 5, :] if is_ctx else g2rep
                            for j in range(2):
                                for f in range(16):
                                    MM(psO[j][0:nr, :], hTt[:, f, r0:r0 + nr], wdn[:, f, j * 512:(j + 1) * 512], f == 0, f == 15,
                                       [f"hTt{f}", f"wdn{f}"], [f"psO{j}"])
                                STT("dve", y_[0:nr, j * 512:(j + 1) * 512], psO[j][0:nr, :], gatg[pe_][0:nr, ci:ci + 1],
                                    gsrc[0:nr, j * 512:(j + 1) * 512], ALU.mult, ALU.mult, [f"psO{j}", f"gatg{pe_}", "mod5", "modc5"], [yk])
                            dst = xc_d if is_ctx else out
                            wkeys = [f"cmb{e_}_{ci}"]
                            rkeys = [f"cmb{e_ - 1}_{cj}" for cj in range(len(tokchunks))]
                            P.op("pool", lambda e, y_=y_, nr=nr, dst=dst, pe_=pe_, ci=ci, is_ctx=is_ctx: e.indirect_dma_start(
                                out=dst, out_offset=bass.IndirectOffsetOnAxis(ap=tokg[pe_][0:nr, ci:ci + 1], axis=0),
                                in_=y_[0:nr, :], in_offset=None, bounds_check=bc(e, (CT if is_ctx else T) - 1), oob_is_err=True,
                                compute_op=ALU.add), [yk, f"tokg{pe_}"] + rkeys, wkeys, dma=True)

                    us = {}
                    us[(0, 0)] = load_unit(0, 0)
                    us[(0, 1)] = load_unit(0, 1)
                    gather_T(0)
                    bg = []
                    bgi = 0
                    for e_ in range(NE):
                        g = e_ // EPG
                        if e_ % EPG == 0:
                            if bgi < len(bg):
                                P.replay(bg[bgi:])
                            bg = dispatch_ops(g + 1) if g + 1 < NG else []
                            bgi = 0
                        per = (len(bg) + 4 * EPG - 1) // (4 * EPG) if bg else 0
                        load_wd(e_)
                        for q in range(4):
                            gateup(e_, q, us[(e_, q)])
                            if bgi < len(bg):
                                P.replay(bg[bgi:bgi + per])
                                bgi += per
                            nq = q + 2
                            if nq < 4:
                                us[(e_, nq)] = load_unit(e_, nq)
                            elif e_ + 1 < NE:
                                us[(e_ + 1, nq - 4)] = load_unit(e_ + 1, nq - 4)
                        for ci in range(len(tokchunks)):
                            down(e_, ci)
                            if e_ + 1 < NE:
                                gather_T(e_ + 1, ci)
                    P.barrier()
                    P.flush()

        for l in range(n_layers):
            with contextlib.ExitStack() as les:
                nvc = 6 if l == 0 else 2
                modc = sbuf(les, "modc", [128, nvc, D], F32)
                modulation2(l, modc, nvc)
                attention(l, modc)
                if not SKIP_MOE:
                    moe(l, modc)
        P.barrier()
        P.flush(final=True)
    return nc


def _rope_tables(T):
    pos = np.arange(T)
    row = (pos // 64).astype(np.float32)
    col = (pos % 64).astype(np.float32)
    inv = (np.float32(10000.0) ** (-np.arange(16, dtype=np.float32) / np.float32(16))).astype(np.float32)
    ar = (row[:, None] * inv).astype(np.float32)
    ac = (col[:, None] * inv).astype(np.float32)
    C = np.concatenate([np.cos(ar), np.cos(ar), np.cos(ac), np.cos(ac)], 1).astype(np.float32)
    S = np.concatenate([-np.sin(ar), np.sin(ar), -np.sin(ac), np.sin(ac)], 1).astype(np.float32)
    return np.ascontiguousarray(C), np.ascontiguousarray(S)


def _mask0():
    kk = np.arange(128)[:, None]
    a = np.arange(128)[None, :]
    m = np.stack([(kk >= a), np.ones((128, 128), bool), (kk <= a)], 1)
    return np.ascontiguousarray(m.reshape(128, 384).astype(np.float32))


def _bias_tab(rpb, T):
    NT = T // 128
    R = T // 64
    reps = [0, 1, 2, NT - 2, NT - 1]
    kk = np.arange(128)
    qq = np.arange(128)
    tab = np.full((5, 128, 16, 5, 128), NEGM, np.float32)
    for v, i in enumerate(reps):
        qrow = 2 * i + qq // 64
        qcol = qq % 64
        rs = np.clip(qrow - 4, 0, R - 8)
        ws = np.clip(qcol - 8, 0, 48)
        dlo_v = [0, -1, -2, -2, -3][v]
        for s in range(5):
            dl = dlo_v + s
            j = i + dl
            if j < 0 or j >= NT:
                continue
            krow = 2 * j + kk // 64
            kcol = kk % 64
            valid = ((krow[:, None] >= rs[None, :]) & (krow[:, None] < rs[None, :] + 8)
                     & (kcol[:, None] >= ws[None, :]) & (kcol[:, None] < ws[None, :] + 16))
            dr = np.clip(krow[:, None] - qrow[None, :] + 7, 0, 14)
            dc = np.clip(kcol[:, None] - qcol[None, :], -15, 15) + 15
            b = rpb[:, dr, dc]
            b = np.where(valid[None], b, NEGM)
            tab[v, :, :, s, :] = np.transpose(b, (1, 0, 2))
    return np.ascontiguousarray(tab.reshape(5, 128, 16 * 5 * 128))


def _lay(v):
    return np.ascontiguousarray(np.asarray(v, np.float32).reshape(KC, 128).T)


_NC_CACHE = {}


def make_in_maps(inp, T):
    f = lambda a: np.ascontiguousarray(np.asarray(a, np.float32))
    B = inp["x"].shape[0]
    C, S = _rope_tables(T)
    shared = dict(
        w_mod=f(inp["w_mod"]), b_mod=f(inp["b_mod"]), norm_mix=f(inp["norm_mix"]), norm_ffn=f(inp["norm_ffn"]),
        a_w_qkv=f(inp["a_w_qkv"][0]), a_q_gain=f(inp["a_q_gain"][0]), a_k_gain=f(inp["a_k_gain"][0]),
        a_sink=f(inp["a_sink"][0]), a_w_o=f(inp["a_w_o"][0]),
        b_w_qkv=f(inp["b_w_qkv"][0]), b_q_gain=f(inp["b_q_gain"][0]), b_k_gain=f(inp["b_k_gain"][0]),
        b_w_o=f(inp["b_w_o"][0]), bias_tab=_bias_tab(f(inp["b_rpb"][0]), T), mask0=_mask0(),
        ropeC=C, ropeS=S, moe_router=f(inp["moe_router"]), moe_w_gate=f(inp["moe_w_gate"]),
        moe_w_up=f(inp["moe_w_up"]), moe_w_down=f(inp["moe_w_down"]),
    )
    maps = []
    for b in range(B):
        m = dict(shared)
        m["x"] = f(inp["x"][b])
        m["ctx"] = f(inp["ctx"][b])
        m["cvec"] = np.ascontiguousarray(np.stack([_lay(inp["c"][b]), _lay(inp["c_ctx"])], 0))
        maps.append(m)
    return maps


def kernel(**inputs):
    T = inputs["x"].shape[1]
    B = inputs["x"].shape[0]
    if T not in _NC_CACHE:
        _NC_CACHE[T] = build(T)
    nc = _NC_CACHE[T]
    maps = make_in_maps(inputs, T)
    res = run_bass_kernel_spmd(nc, maps, core_ids=list(range(B)))
    return np.stack([np.asarray(r["out"], np.float32) for r in res.results], 0)
```

```python
import contextlib
import numpy as np
import concourse.bass as bass
import concourse.mybir as mybir
from concourse.bass_utils import run_bass_kernel_spmd

F32 = mybir.dt.float32
BF16 = mybir.dt.bfloat16
I32 = mybir.dt.int32
ALU = mybir.AluOpType
AF = mybir.ActivationFunctionType
AX = mybir.AxisListType

D = 1024
KC = 8
NE = 16
FF = 2048
CT = 256
EPS = 1e-6
SCALE = 0.125
BIG = float(2 ** 20)
ROWW = 1041
CCAP = 32
NOALIAS = False
DEFER_POST = 1
SKIP_MOE = False
STORE_FG = False
DEBUG = False
NEGM = -30000.0


class Prog:
    ENGS = ("pe", "act", "dve", "pool", "sp")
    NDMASEM = 16

    def __init__(self, nc):
        self.nc = nc
        self.ops = []
        self.last_w = {}
        self.readers = {}
        self.last_eng = {}
        self.dmas = []
        self.emitted = 0
        self.stack = contextlib.ExitStack()
        self.sems = {}
        for e in self.ENGS:
            self.sems[(e, "c")] = self.stack.enter_context(nc.semaphore(f"c_{e}"))
        for e in ("sp", "pool"):
            for k in range(self.NDMASEM):
                self.sems[(e, k)] = self.stack.enter_context(nc.semaphore(f"d_{e}_{k}"))
        self.ms = {e: 0 for e in self.ENGS}
        self.rr = {e: 0 for e in self.ENGS}
        self.dcnt = {}
        self.waited = {e: {} for e in self.ENGS}

    def defer_begin(self):
        self._defer = []

    def defer_end(self):
        d, self._defer = self._defer, None
        return d

    def replay(self, items):
        for it in items:
            if it is not None:
                self.op(*it)

    def mark(self):
        if getattr(self, "_defer", None) is not None:
            self._defer.append(None)

    def op(self, eng, fn, reads=(), writes=(), dma=False, extra=(), force=False):
        if getattr(self, "_defer", None) is not None:
            self._defer.append((eng, fn, tuple(reads), tuple(writes), dma, tuple(extra), force))
            return None
        idx = len(self.ops)
        deps = set(extra)
        for r in reads:
            if r in self.last_w:
                deps.add(self.last_w[r])
        for w in writes:
            if w in self.last_w:
                deps.add(self.last_w[w])
            deps.update(self.readers.get(w, ()))
        for r in reads:
            self.readers.setdefault(r, []).append(idx)
        for w in writes:
            self.last_w[w] = idx
            self.readers[w] = []
        deps.discard(idx)
        self.ops.append(dict(eng=eng, fn=fn, deps=sorted(deps), dma=dma, idx=idx, force=force))
        if dma:
            self.dmas.append(idx)
        else:
            self.last_eng[eng] = idx
        return idx

    def barrier(self):
        deps = list(self.last_eng.values()) + list(self.dmas)
        for e in self.ENGS:
            self.op(e, lambda eng: eng.nop(), extra=deps, force=True)
        self.dmas = []
        self.last_w = {}
        self.readers = {}

    SAME_ENG_SYNC = True

    @classmethod
    def _skip(cls, a, o):
        if a["dma"] or o["dma"]:
            return False
        if a["eng"] != o["eng"]:
            return False
        return a["eng"] == "pe" or not cls.SAME_ENG_SYNC

    def flush(self, final=False):
        nc = self.nc
        ops = self.ops
        batch = ops[self.emitted:]
        base = self.emitted
        needed = set()
        for o in batch:
            if o["force"]:
                needed.add(o["idx"])
            for d in o["deps"]:
                if not self._skip(ops[d], o):
                    if d < base:
                        assert "sem" in ops[d], "cross-phase dependency on unsignalled op"
                    needed.add(d)
        for o in batch:
            if o["dma"]:
                key = (o["eng"], self.rr[o["eng"]] % self.NDMASEM)
                self.rr[o["eng"]] += 1
                prev = self.dcnt.get(key, 0)
                self.dcnt[key] = prev + 16
                o["sem"], o["val"], o["prev"] = key, prev + 16, prev
            elif o["idx"] in needed:
                self.ms[o["eng"]] += 1
                o["sem"], o["val"] = (o["eng"], "c"), self.ms[o["eng"]]
        self.emitted = len(ops)
        sems = self.sems
        engobj = {"pe": "tensor", "act": "scalar", "dve": "vector", "pool": "gpsimd", "sp": "sync"}
        with nc.Block() as block:
            for e in self.ENGS:
                myops = [o for o in batch if o["eng"] == e]

                def body(eng, myops=myops, e=e):
                    waited = self.waited[e]
                    for o in myops:
                        need = {}
                        for d in o["deps"]:
                            a = ops[d]
                            if self._skip(a, o):
                                continue
                            if a["val"] > need.get(a["sem"], 0):
                                need[a["sem"]] = a["val"]
                        if o["dma"] and o["prev"] > 0:
                            need[o["sem"]] = max(need.get(o["sem"], 0), o["prev"])
                        for k, v in need.items():
                            if waited.get(k, 0) < v:
                                waited[k] = v
                                eng.wait_ge(sems[k], v)
                        ins = o["fn"](eng)
                        if "sem" in o:
                            ins.then_inc(sems[o["sem"]], 16 if o["dma"] else 1)
                    if final:
                        for (ee, k), v in self.dcnt.items():
                            if ee == e and waited.get((ee, k), 0) < v:
                                waited[(ee, k)] = v
                                eng.wait_ge(sems[(ee, k)], v)
                getattr(block, engobj[e])(body)
        if final:
            self.stack.close()


def build(T, n_layers=2):
    NT = T // 128
    NCT = CT // 128
    cap = T // 8
    XR = cap + 128
    nc = bass.Bass("TRN2", target_bir_lowering=False)
    P = Prog(nc)

    def din(name, shape, dt=F32):
        return nc.dram_tensor(name, shape, dt, kind="ExternalInput").ap()

    x_in = din("x", [T, D])
    ctx_in = din("ctx", [CT, D])
    cvec = din("cvec", [2, 128, KC])
    w_mod = din("w_mod", [2, D, 6 * D])
    b_mod = din("b_mod", [2, 6 * D])
    norm_mix = din("norm_mix", [2, D])
    norm_ffn = din("norm_ffn", [2, D])
    wqkv_d = [din("a_w_qkv", [D, 1536]), din("b_w_qkv", [D, 3072])]
    qg_d = [din("a_q_gain", [64]), din("b_q_gain", [64])]
    kg_d = [din("a_k_gain", [64]), din("b_k_gain", [64])]
    sink_d = din("a_sink", [16])
    wo_d = [din("a_w_o", [D, D]), din("b_w_o", [D, D])]
    bias_tab = din("bias_tab", [5, 128, 16 * 5 * 128])
    mask0_d = din("mask0", [128, 384])
    ropeC = din("ropeC", [T, 64])
    ropeS = din("ropeS", [T, 64])
    router_d = din("moe_router", [2, D, NE])
    wg_d = din("moe_w_gate", [2, NE, D, FF])
    wu_d = din("moe_w_up", [2, NE, D, FF])
    wd_d = din("moe_w_down", [2, NE, FF, D])
    out = nc.dram_tensor("out", [T, D], F32, kind="ExternalOutput").ap()
    xc_d = nc.dram_tensor("xc_d", [CT, D], F32, kind=("ExternalOutput" if DEBUG else "Internal")).ap()
    h2_d = nc.dram_tensor("h2_d", [T + CT, ROWW], F32).ap()
    aff_d = nc.dram_tensor("aff_d", [T + CT, NE], F32).ap()
    xg_d = [nc.dram_tensor(f"xg_d{e}", [XR, ROWW], F32).ap() for e in range(NE)]
    eb_d = nc.dram_tensor("eb_d", [5, 128, 16 * 5 * 128], BF16).ap()

    uid = [0]

    def sbuf(es, name, shape, dt):
        uid[0] += 1
        return es.enter_context(nc.sbuf_tensor(f"{name}_{uid[0]}", shape, dt))

    def psum(es, name, shape, dt):
        return es.enter_context(nc.psum_tensor(name, shape, dt))

    def MM(o, l, r_, st, sp_, R, W):
        P.op("pe", lambda e: e.matmul(o, lhsT=l, rhs=r_, start=st, stop=sp_), R, W)

    def TR(o, i, idn, R, W):
        P.op("pe", lambda e: e.transpose(o, in_=i, identity=idn), R, W)

    def ACT(o, i, func, R, W, scale=1.0, bias=0.0, accum=None):
        if accum is None:
            P.op("act", lambda e: e.activation(out=o, in_=i, func=func, bias=bias, scale=scale), R, W)
        else:
            P.op("act", lambda e: e.activation(out=o, in_=i, func=func, bias=bias, scale=scale, accum_out=accum), R, W)

    def TT(eng, o, a, b, op, R, W):
        P.op(eng, lambda e: e.tensor_tensor(out=o, in0=a, in1=b, op=op), R, W)

    def TS(eng, o, a, s1, s2, op0, op1, R, W, accum=None):
        if accum is None:
            P.op(eng, lambda e: e.tensor_scalar(out=o, in0=a, scalar1=s1, scalar2=s2, op0=op0, op1=op1), R, W)
        else:
            P.op(eng, lambda e: e.tensor_scalar(out=o, in0=a, scalar1=s1, scalar2=s2, op0=op0, op1=op1, accum_out=accum), R, W)

    def STT(eng, o, a, s, b, op0, op1, R, W):
        P.op(eng, lambda e: e.scalar_tensor_tensor(out=o, in0=a, scalar=s, in1=b, op0=op0, op1=op1), R, W)

    def CP(eng, o, i, R, W):
        if eng == "act":
            P.op("act", lambda e: e.copy(out=o, in_=i), R, W)
        else:
            P.op(eng, lambda e: e.tensor_copy(out=o, in_=i), R, W)

    def RECIP(o, i, R, W):
        P.op("dve", lambda e: e.reciprocal(out=o, in_=i), R, W)

    def DMA(eng, o, i, R, W):
        P.op(eng, lambda e: e.dma_start(out=o, in_=i), R, W, dma=True)

    bcregs = {}

    def bc(e, val):
        if val not in bcregs:
            r = e.alloc_register(f"bc{val}")
            e.reg_mov(r, val)
            bcregs[val] = r
        return bcregs[val]

    def MEMSET(eng, o, v, W):
        P.op(eng, lambda e: e.memset(o, v), (), W)

    with contextlib.ExitStack() as top:
        ident = sbuf(top, "ident", [128, 128], BF16)
        identf = sbuf(top, "identf", [128, 128], F32)
        modrep = sbuf(top, "modrep", [128, 6, D], F32)
        psT = psum(top, "psT", [128, 8, 128], BF16)
        psW = [psum(top, f"psW{i}", [128, 512], F32) for i in range(2)]
        psS = [psum(top, f"psS{i}", [128, 512], F32) for i in range(2)]
        psO = [psum(top, f"psO{i}", [128, 512], F32) for i in range(3)]

        MEMSET("pool", ident[:], 0.0, ["ident"])
        P.op("pool", lambda e: e.affine_select(out=ident[:], in_=ident[:], pattern=[[-1, 128]],
                                               compare_op=ALU.not_equal, fill=1.0, base=0, channel_multiplier=1),
             ["ident"], ["ident"])
        MEMSET("pool", identf[:], 0.0, ["identf"])
        P.op("pool", lambda e: e.affine_select(out=identf[:], in_=identf[:], pattern=[[-1, 128]],
                                               compare_op=ALU.not_equal, fill=1.0, base=0, channel_multiplier=1),
             ["identf"], ["identf"])

        if n_layers > 1:
            with contextlib.ExitStack() as es:
                bt = [sbuf(es, f"bt{i}", [128, 2048], F32) for i in range(2)]
                bo = [sbuf(es, f"bo{i}", [128, 2048], BF16) for i in range(2)]
                n = 0
                for v in range(5):
                    for c0 in range(0, 10240, 2048):
                        i = n % 2
                        n += 1
                        DMA("sp", bt[i][:], bias_tab[v, :, c0:c0 + 2048], [], [f"bt{i}"])
                        ACT(bo[i][:], bt[i][:], AF.Exp, [f"bt{i}"], [f"bo{i}"])
                        DMA("sp", eb_d[v, :, c0:c0 + 2048], bo[i][:], [f"bo{i}"], ["eb_d"])
                P.barrier()
                P.flush()

        def modulation2(l, modc, nvc):
            with contextlib.ExitStack() as es:
                cv = sbuf(es, "cv", [128, 2, KC], F32)
                sc = sbuf(es, "sc", [128, 2, KC], F32)
                lrep = sbuf(es, "lrep", [128, 2, KC, 128], F32)
                wm = [sbuf(es, f"wm{i}", [128, KC, 512], F32) for i in range(2)]
                brep = sbuf(es, "brep", [128, 6 * D], F32)
                nrep = sbuf(es, "nrep", [128, 2, D], F32)
                raws = [sbuf(es, f"modraw{w}", [128, 6, D], F32) for w in range(2)]
                for w in range(2):
                    DMA("sp", cv[:, w, :], cvec[w], [], [f"cv{w}"])
                DMA("sp", brep[:], b_mod[l].partition_broadcast(128), [], ["brep"])
                DMA("sp", nrep[:, 0, :], norm_mix[l].partition_broadcast(128), [], ["nrep0"])
                DMA("sp", nrep[:, 1, :], norm_ffn[l].partition_broadcast(128), [], ["nrep1"])
                for w in range(2):
                    ACT(sc[:, w, :], cv[:, w, :], AF.Silu, [f"cv{w}"], [f"sc{w}"])
                    for k in range(KC):
                        CP("dve", lrep[:, w, k, :], sc[:, w, k:k + 1].to_broadcast([128, 128]), [f"sc{w}"], [f"lrep{w}_{k}"])
                wsrc = w_mod[l].rearrange("(k p) n -> p k n", p=128)
                for j in range(12):
                    i = j % 2
                    DMA("sp", wm[i][:], wsrc[:, :, j * 512:(j + 1) * 512], [], [f"wm{i}"])
                    for w in range(2):
                        ps_, pk_ = (psW[i], f"psW{i}") if w == 0 else (psS[i], f"psS{i}")
                        for k in range(KC):
                            MM(ps_[:], lrep[:, w, k, :], wm[i][:, k, :], k == 0, k == KC - 1,
                               [f"lrep{w}_{k}", f"wm{i}"], [pk_])
                        mflat = raws[w][:].rearrange("p a d -> p (a d)")
                        TT("dve", mflat[:, j * 512:(j + 1) * 512], ps_[:], brep[:, j * 512:(j + 1) * 512], ALU.add,
                           [pk_, "brep"], [f"raw{w}_{j // 2}"])
                for w in range(2):
                    STT("dve", raws[w][:, 1, :], raws[w][:, 1, :], 1.0, nrep[:, 0, :], ALU.add, ALU.mult,
                        [f"raw{w}_1", "nrep0"], [f"raw{w}_1"])
                    STT("dve", raws[w][:, 4, :], raws[w][:, 4, :], 1.0, nrep[:, 1, :], ALU.add, ALU.mult,
                        [f"raw{w}_4", "nrep1"], [f"raw{w}_4"])
                for v in range(6):
                    CP("pool" if v % 2 else "dve", modrep[:, v, :], raws[0][:, v, :], [f"raw0_{v}"], [f"mod{v}"])
                for v in range(nvc):
                    CP("dve" if v % 2 else "pool", modc[:, v, :], raws[1][:, v, :], [f"raw1_{v}"], [f"modc{v}"])
                P.barrier()
                P.flush()

        MODKEYS = [f"mod{i}" for i in range(6)]

        def attention(l, modc):
            nkv = 4 if l == 0 else 16
            NQKV = (16 + 2 * nkv) * 64
            koff = 1024
            voff = 1024 + nkv * 64
            W = 1 if l == 0 else 2
            NS = 2 * W + 2
            NHS = W + 2
            nkp = 4 if l == 0 else 8
            rope = (l == 0)
            last = (l == n_layers - 1)
            with contextlib.ExitStack() as es:
                wqkv = sbuf(es, "wqkv", [128, KC, NQKV], BF16)
                wo = sbuf(es, "wo", [128, KC, D], BF16)
                wr = sbuf(es, "wr", [128, KC, NE], BF16)
                qg = sbuf(es, "qg", [128, 64], F32)
                kg = sbuf(es, "kg", [128, 64], F32)
                sinke = sbuf(es, "sinke", [128, 16], F32)
                mhalf = sbuf(es, "mhalf", [128, 16], F32)
                KT = sbuf(es, "KT", [128, NS + NCT, nkp, 128], BF16)
                VA = sbuf(es, "VA", [128, NS + NCT, nkv, 65], BF16)
                hT = sbuf(es, "hT", [128, D], BF16)
                xt = sbuf(es, "xt", [128, D], F32)
                Sa = sbuf(es, "Sa", [128, D], F32)
                Sc = sbuf(es, "Sc", [128, D], F32)
                hb = sbuf(es, "hb", [128, D], BF16)
                qn = sbuf(es, "qn", [128, D], BF16)
                kn = sbuf(es, "kn", [128, D], BF16)
                QTr = sbuf(es, "QTr", [128, NHS + (NCT if l == 0 else 0), 8, 128], BF16)
                Pb = [sbuf(es, f"Pb{i}", [128, 896], BF16) for i in range(2)]
                Osb = sbuf(es, "Osb", [128, D], BF16)
                OT = sbuf(es, "OT", [128, D], BF16)
                h2a = sbuf(es, "h2a", [128, ROWW], F32)
                tidi = sbuf(es, "tidi", [128, 1], I32)
                h2T = sbuf(es, "h2T", [128, D], BF16)
                afft = sbuf(es, "afft", [128, NE], F32)
                if l == 0:
                    mtab = sbuf(es, "mtab", [128, 384], BF16)
                else:
                    mtab = sbuf(es, "mtab", [128, 16, 640], BF16)

                def scratch(tag, full):
                    s = dict(tag=tag)
                    s["sm"] = sbuf(es, "sm" + tag, [128, 128], F32)
                    s["nsq"] = sbuf(es, "nsq" + tag, [128, 512], F32)
                    s["nt1"] = sbuf(es, "nt1" + tag, [128, 512], F32)
                    if rope:
                        for nm in ("nt2", "nt3", "nt4"):
                            s[nm] = sbuf(es, nm + tag, [128, 512], F32)
                        s["rC"] = sbuf(es, "rC" + tag, [128, 64], F32)
                        s["rS"] = sbuf(es, "rS" + tag, [128, 64], F32)
                    return s

                scA = dict(tag="A")
                scA["sm"] = sbuf(es, "smA", [128, 128], F32)
                scA["nsq"] = sbuf(es, "nsqA", [128, 512], F32)
                scA["nt1"] = sbuf(es, "nt1A", [128, 512], F32)
                scA["Sa"] = Sa
                scA["hb"] = hb
                scP = dict(tag="P")
                scP["sm"] = sbuf(es, "smP", [128, 128], F32)
                scP["nt1"] = sbuf(es, "nt1P", [128, 512], F32)
                if rope:
                    for s_ in (scA, scP):
                        for nm in ("nt2", "nt3", "nt4"):
                            s_[nm] = sbuf(es, nm + s_["tag"], [128, 512], F32)
                        s_["rC"] = sbuf(es, "rC" + s_["tag"], [128, 64], F32)
                        s_["rS"] = sbuf(es, "rS" + s_["tag"], [128, 64], F32)
                if l == 0 or NOALIAS:
                    scP["nsq"] = sbuf(es, "nsqP", [128, 512], F32)
                    scP["Sa"] = sbuf(es, "SaP", [128, D], F32)
                    scP["hb"] = sbuf(es, "hbP", [128, D], BF16)
                else:
                    scP["Sa"] = modc[:, 0, :]
                    scP["hb"] = modc[:, 1, 0:512].bitcast(BF16)
                    scP["nsq"] = modc[:, 1, 512:1024]

                WQK = [f"wqkv{k}_{n0}" for k in range(KC) for n0 in range(0, NQKV, 1024)]
                for k in range(KC):
                    for n0 in range(0, NQKV, 1024):
                        n1 = min(NQKV, n0 + 1024)
                        DMA("pool", wqkv[:, k, n0:n1], wqkv_d[l][k * 128:(k + 1) * 128, n0:n1], [], [f"wqkv{k}_{n0}"])
                    DMA("pool", wo[:, k, :], wo_d[l][k * 128:(k + 1) * 128, :], [], [f"wo{k}"])
                DMA("pool", wr[:], router_d[l].rearrange("(k p) e -> p k e", p=128), [], ["wr"])
                DMA("sp", qg[:], qg_d[l].partition_broadcast(128), [], ["qg"])
                DMA("sp", kg[:], kg_d[l].partition_broadcast(128), [], ["kg"])
                MEMSET("pool", mhalf[:], -0.5, ["mhalf"])
                if l == 0:
                    DMA("sp", sinke[:], sink_d.partition_broadcast(128), [], ["sinke"])
                    ACT(sinke[:], sinke[:], AF.Exp, ["sinke"], ["sinke"])
                    DMA("pool", mtab[:], mask0_d, [], ["mtab"])
                else:
                    MEMSET("pool", sinke[:], 0.0, ["sinke"])
                MEMSET("pool", VA[:, :, :, 64:65], 1.0, ["VAones"])

                def rstd_of(sc_, src_ap, dst_ap, n, inv, kin, kout):
                    t = sc_["tag"]
                    TS("pool", dst_ap, src_ap, inv, EPS, ALU.mult, ALU.add, [kin], [kout + "_t"])
                    TT("pool", dst_ap, dst_ap, mhalf[:, 0:n], ALU.pow, [kout + "_t", "mhalf"], [kout])

                def norm_seg(sc_, src, nh, gain, keyg, use_rope, dst, R, Wk):
                    t = sc_["tag"]
                    sm = sc_["sm"]
                    nsq, nt1 = sc_["nsq"], sc_["nt1"]
                    n = nh * 64
                    s3 = src.rearrange("p (h d) -> p h d", d=64)
                    ACT(nsq[:, 0:n], src, AF.Square, R, ["nsq" + t])
                    P.op("dve", lambda e: e.reduce_sum(out=sm[:, 0:nh], in_=nsq[:, 0:n].rearrange("p (h d) -> p h d", d=64), axis=AX.X),
                         ["nsq" + t], ["ssq" + t])
                    rstd_of(sc_, sm[:, 0:nh], sm[:, 32:32 + nh], nh, 1.0 / 64, "ssq" + t, "rs" + t)
                    t1 = nt1[:, 0:n].rearrange("p (h d) -> p h d", d=64)
                    TT("dve", t1, s3, gain[:].unsqueeze(1).to_broadcast([128, nh, 64]), ALU.mult, R + [keyg], ["nt1" + t])
                    rsb = sm[:, 32:32 + nh].unsqueeze(2).to_broadcast([128, nh, 64])
                    if not use_rope:
                        TT("dve", dst, t1, rsb, ALU.mult, ["nt1" + t, "rs" + t], Wk)
                        return
                    nt2, nt3, nt4, rC, rS = sc_["nt2"], sc_["nt3"], sc_["nt4"], sc_["rC"], sc_["rS"]
                    t2 = nt2[:, 0:n].rearrange("p (h d) -> p h d", d=64)
                    TT("dve", t2, t1, rsb, ALU.mult, ["nt1" + t, "rs" + t], ["nt2" + t])
                    t3 = nt3[:, 0:n].rearrange("p (h d) -> p h d", d=64)
                    TT("pool", t3, t2, rC[:].unsqueeze(1).to_broadcast([128, nh, 64]), ALU.mult, ["nt2" + t, "rC" + t], ["nt3" + t])
                    t2v = nt2[:, 0:n].rearrange("p (h a g d) -> p h a g d", a=2, g=2, d=16)
                    t4v = nt4[:, 0:n].rearrange("p (h a g d) -> p h a g d", a=2, g=2, d=16)
                    rSv = rS[:].rearrange("p (a g d) -> p a g d", a=2, g=2)
                    TT("dve", t4v[:, :, :, 0, :], t2v[:, :, :, 1, :],
                       rSv[:, :, 0, :].unsqueeze(1).to_broadcast([128, nh, 2, 16]), ALU.mult, ["nt2" + t, "rS" + t], ["nt4a" + t])
                    TT("dve", t4v[:, :, :, 1, :], t2v[:, :, :, 0, :],
                       rSv[:, :, 1, :].unsqueeze(1).to_broadcast([128, nh, 2, 16]), ALU.mult, ["nt2" + t, "rS" + t], ["nt4b" + t])
                    t4 = nt4[:, 0:n].rearrange("p (h d) -> p h d", d=64)
                    TT("dve", dst, t3, t4, ALU.add, ["nt3" + t, "nt4a" + t, "nt4b" + t], Wk)

                def prenorm(sc_, src_t, keysrc, gi, dst_bf, keydst, mod, mk, dst32=None, key32=None):
                    t = sc_["tag"]
                    sm = sc_["sm"]
                    Sx = sc_["Sa"]
                    ACT(Sx[:], src_t, AF.Square, [keysrc], ["Sa" + t, "ss" + t], accum=sm[:, 64:65])
                    rstd_of(sc_, sm[:, 64:65], sm[:, 66:67], 1, 1.0 / D, "ss" + t, "r" + t)
                    STT("dve", Sx[:], src_t, sm[:, 66:67], mod[:, gi + 1, :], ALU.mult, ALU.mult,
                        [keysrc, "r" + t, f"{mk}{gi + 1}"], ["Sa" + t])
                    TT("dve", dst_bf, Sx[:], mod[:, gi, :], ALU.add, ["Sa" + t, f"{mk}{gi}"], [keydst])
                    if dst32 is not None:
                        TT("pool", dst32, Sx[:], mod[:, gi, :], ALU.add, ["Sa" + t, f"{mk}{gi}"], [key32])

                def tile_info(kind, i):
                    if kind == "l":
                        src = (x_in if l == 0 else out)[i * 128:(i + 1) * 128, :]
                        return src, f"xl{i}", i % NS, i % NHS, out[i * 128:(i + 1) * 128, :]
                    src = (ctx_in if l == 0 else xc_d)[i * 128:(i + 1) * 128, :]
                    return src, f"xc{i}", NS + i, NHS + (i if l == 0 else 0), xc_d[i * 128:(i + 1) * 128, :]

                def project(kind, i):
                    src, dkey, slot, hs, _ = tile_info(kind, i)
                    use_rope = rope and kind == "l"
                    sc_ = scP if kind == "l" else scA
                    t = sc_["tag"]
                    hbx = sc_["hb"]
                    mod, mk = (modrep, "mod") if kind == "l" else (modc, "modc")
                    DMA("sp", xt[:], src, [dkey], ["xt"])
                    if use_rope:
                        DMA("sp", sc_["rC"][:], ropeC[i * 128:(i + 1) * 128, :], [], ["rC" + t])
                        DMA("sp", sc_["rS"][:], ropeS[i * 128:(i + 1) * 128, :], [], ["rS" + t])
                    prenorm(sc_, xt[:], "xt", 0, hbx[:], "hb" + t, mod, mk)
                    for k in range(KC):
                        TR(psT[:, k, :], hbx[:, k * 128:(k + 1) * 128], ident[:], ["hb" + t, "ident"], ["psT"])
                    CP("act", hT[:], psT[:].rearrange("p k t -> p (k t)"), ["psT"], ["hT"])
                    P.mark()
                    hTv = hT[:].rearrange("p (k t) -> p k t", k=KC)
                    if kind == "l" or l == 0:
                        for j in range(2):
                            pw = psW[j]
                            for k in range(KC):
                                MM(pw[:], hTv[:, k, :], wqkv[:, k, j * 512:(j + 1) * 512], k == 0, k == KC - 1,
                                   ["hT"] + WQK, [f"psW{j}"])
                            norm_seg(sc_, pw[:], 8, qg, "qg", use_rope,
                                     qn[:, j * 512:(j + 1) * 512].rearrange("p (h d) -> p h d", d=64), [f"psW{j}"], [f"qn{j}"])
                            P.mark()
                        for j in range(8):
                            TR(psT[:, j, :], qn[:, j * 128:(j + 1) * 128], ident[:], ["qn0", "qn1", "ident"], ["psT"])
                        CP("act", QTr[:, hs, :, :], psT[:], ["psT"], [f"QT{hs}"])
                        P.mark()
                    c0 = koff
                    ci = 0
                    while c0 < NQKV:
                        c1 = min(NQKV, c0 + 512)
                        pw = psW[ci % 2]
                        pk = f"psW{ci % 2}"
                        ci += 1
                        for k in range(KC):
                            MM(pw[:, 0:c1 - c0], hTv[:, k, :], wqkv[:, k, c0:c1], k == 0, k == KC - 1,
                               ["hT"] + WQK, [pk])
                        a0, a1 = max(c0, koff), min(c1, voff)
                        if a1 > a0:
                            nh = (a1 - a0) // 64
                            h0 = (a0 - koff) // 64
                            if l == 0:
                                dst = kn[:, 0:512].rearrange("p (h a d) -> p h a d", a=2, d=64)[:, :, 0, :]
                            else:
                                dst = kn[:, h0 * 64:(h0 + nh) * 64].rearrange("p (h d) -> p h d", d=64)
                            norm_seg(sc_, pw[:, a0 - c0:a1 - c0], nh, kg, "kg", use_rope, dst, [pk], [f"kn{h0 // 8}"])
                        a0, a1 = max(c0, voff), min(c1, NQKV)
                        if a1 > a0:
                            nh = (a1 - a0) // 64
                            h0 = (a0 - voff) // 64
                            CP("act", VA[:, slot, h0:h0 + nh, 0:64],
                               pw[:, a0 - c0:a1 - c0].rearrange("p (h d) -> p h d", d=64), [pk], [f"VA{slot}_{h0 // 8}"])
                        c0 = c1
                        P.mark()
                    if l == 0:
                        knv = kn[:, 0:512].rearrange("p (h a d) -> p h a d", a=2, d=64)
                        CP("pool", knv[:, :, 1, :], knv[:, :, 0, :], ["kn0"], ["kn0b"])
                        for j in range(4):
                            TR(psT[:, j, :], kn[:, j * 128:(j + 1) * 128], ident[:], ["kn0", "kn0b", "ident"], ["psT"])
                    else:
                        for j in range(8):
                            TR(psT[:, j, :], kn[:, j * 128:(j + 1) * 128], ident[:], ["kn0", "kn1", "ident"], ["psT"])
                    CP("act", KT[:, slot, :, :], psT[:, 0:nkp, :], ["psT"], [f"KT{slot}"])

                cur = dict(var=-1)

                def heads(kind, i, pending):
                    src, dkey, slot, hs, dstrows = tile_info(kind, i)
                    sm = scA["sm"]
                    QT = QTr[:, hs, :, :]
                    qtk = f"QT{hs}"
                    chunks = []
                    if kind == "l":
                        dlo = max(-W, -i)
                        dhi = min(W, NT - 1 - i)
                        if l == 1:
                            if i == 0:
                                dlo, dhi = 0, min(3, NT - 1)
                            elif i == NT - 1:
                                dlo, dhi = max(-3, -i), 0
                        for dl in range(dlo, dhi + 1):
                            chunks.append((i + dl) % NS)
                        nl = len(chunks)
                    else:
                        nl = 0
                        dlo = 0
                    chunks += [NS + j for j in range(NCT)]
                    nch = len(chunks)
                    nA = min(4, nch)
                    nB = nch - nA
                    if kind == "l" and l == 1:
                        v = 0 if i == 0 else 1 if i == 1 else 3 if i == NT - 2 else 4 if i == NT - 1 else 2
                        if cur["var"] != v:
                            cur["var"] = v
                            DMA("sp", mtab[:].rearrange("p h c -> p (h c)"), eb_d[v], ["eb_d"], ["mtab"])

                    def st_phase(h):
                        rows = slice(64 * (h % 2), 64 * (h % 2) + 64)
                        kidx = h // 2 if l == 1 else h // 4
                        hp = h % 2
                        pb = Pb[hp]
                        for c, sl in enumerate(chunks):
                            g = c // 4
                            MM(psS[g][:, (c % 4) * 128:(c % 4 + 1) * 128], KT[rows, sl, kidx, :], QT[rows, h // 2, :],
                               True, True, [f"KT{sl}", qtk], [f"psS{g}"])
                        ACT(pb[:, 0:nA * 128], psS[0][:, 0:nA * 128], AF.Exp, ["psS0"], [f"Pb{hp}a"], scale=SCALE)
                        if nB:
                            ACT(pb[:, 512:512 + nB * 128], psS[1][:, 0:nB * 128], AF.Exp, ["psS1"], [f"Pb{hp}b"], scale=SCALE)
                        if nl:
                            if l == 0:
                                mt = mtab[:, (dlo + W) * 128:(dlo + W + nl) * 128]
                            else:
                                mt = mtab[:, h, 0:nl * 128]
                            TT("dve", pb[:, 0:nl * 128], pb[:, 0:nl * 128], mt, ALU.mult,
                               [f"Pb{hp}a", f"Pb{hp}b", "mtab"], [f"Pb{hp}a", f"Pb{hp}b"])

                    def pv_phase(h):
                        kvh = h if l == 1 else h // 4
                        hp = h % 2
                        pb = Pb[hp]
                        og = h // 6
                        hh = h % 6
                        for c, sl in enumerate(chunks):
                            MM(psO[og][:, hh * 65:(hh + 1) * 65], pb[:, c * 128:(c + 1) * 128], VA[:, sl, kvh, :],
                               c == 0, c == nch - 1,
                               [f"Pb{hp}a", f"Pb{hp}b", f"VA{sl}_{kvh // 8}", "VAones"], [f"psO{og}"])

                    npend = len(pending)
                    per = (npend + 14) // 15 if npend else 0
                    pi = 0
                    st_phase(0)
                    for h in range(16):
                        if h + 1 < 16:
                            st_phase(h + 1)
                        pv_phase(h)
                        if pi < npend:
                            P.replay(pending[pi:pi + per])
                            pi += per
                    if pi < npend:
                        P.replay(pending[pi:])
                    for og in range(3):
                        h0 = og * 6
                        ng = min(6, 16 - h0)
                        pv = psO[og][:, 0:ng * 65].rearrange("p (h d) -> p h d", d=65)
                        TT("dve", sm[:, 80:80 + ng], pv[:, :, 64], sinke[:, h0:h0 + ng], ALU.add,
                           [f"psO{og}", "sinke"], ["sm_den"])
                        RECIP(sm[:, 96:96 + ng], sm[:, 80:80 + ng], ["sm_den"], ["sm_rden"])
                        TT("dve", Osb[:, h0 * 64:(h0 + ng) * 64].rearrange("p (h d) -> p h d", d=64), pv[:, :, 0:64],
                           sm[:, 96:96 + ng].unsqueeze(2).to_broadcast([128, ng, 64]), ALU.mult,
                           [f"psO{og}", "sm_rden"], ["Osb"])

                def post(kind, i, part=7):
                    src, dkey, slot, hs, dstrows = tile_info(kind, i)
                    if part & 1:
                        post_a(kind, i, 1)
                    if part & 4:
                        post_a(kind, i, 2)
                    if part & 2:
                        post_b(kind, i)

                def post_a(kind, i, sub):
                    src, dkey, slot, hs, dstrows = tile_info(kind, i)
                    mod, mk = (modrep, "mod") if kind == "l" else (modc, "modc")
                    if sub == 1:
                        DMA("sp", Sc[:], src, [dkey], ["Sc"])
                        for k in range(KC):
                            TR(psT[:, k, :], Osb[:, k * 128:(k + 1) * 128], ident[:], ["Osb", "ident"], ["psT"])
                        CP("act", OT[:], psT[:].rearrange("p k t -> p (k t)"), ["psT"], ["OT"])
                        P.mark()
                        return
                    OTv = OT[:].rearrange("p (k t) -> p k t", k=KC)
                    for j in range(2):
                        for k in range(KC):
                            MM(psW[j][:], OTv[:, k, :], wo[:, k, j * 512:(j + 1) * 512], k == 0, k == KC - 1,
                               ["OT", f"wo{k}"], [f"psW{j}"])
                        TT("dve", Sa[:, j * 512:(j + 1) * 512], psW[j][:], mod[:, 2, j * 512:(j + 1) * 512], ALU.mult,
                           [f"psW{j}", f"{mk}2"], ["SaA"])
                    TT("dve", Sc[:], Sc[:], Sa[:], ALU.add, ["Sc", "SaA"], ["Sc"])
                    P.mark()
                    if not (last and kind == "c") and not (STORE_FG and getattr(P, "_defer", None) is not None):
                        DMA("sp", dstrows, Sc[:], ["Sc"], [dkey])

                def post_store(kind, i):
                    src, dkey, slot, hs, dstrows = tile_info(kind, i)
                    DMA("sp", dstrows, Sc[:], ["Sc"], [dkey])

                def post_b(kind, i):
                    src, dkey, slot, hs, dstrows = tile_info(kind, i)
                    do_moe = (kind == "l") or (l == 0)
                    mod, mk = (modrep, "mod") if kind == "l" else (modc, "modc")
                    sm = scA["sm"]
                    if not do_moe:
                        return
                    prenorm(scA, Sc[:], "Sc", 3, hb[:], "hbA", mod, mk, h2a[:, 0:D], "h2a")
                    for k in range(KC):
                        TR(psT[:, k, :], hb[:, k * 128:(k + 1) * 128], ident[:], ["hbA", "ident"], ["psT"])
                    CP("act", h2T[:], psT[:].rearrange("p k t -> p (k t)"), ["psT"], ["h2T"])
                    P.mark()
                    h2Tv = h2T[:].rearrange("p (k t) -> p k t", k=KC)
                    for k in range(KC):
                        MM(psW[0][:, 0:NE], h2Tv[:, k, :], wr[:, k, :], k == 0, k == KC - 1, ["h2T", "wr"], ["psW0"])
                    ACT(sm[:, 104:120], psW[0][:, 0:NE], AF.Exp, ["psW0"], ["sm_ex", "sm_es"], accum=sm[:, 120:121])
                    RECIP(sm[:, 121:122], sm[:, 120:121], ["sm_es"], ["sm_er"])
                    TS("dve", afft[:], sm[:, 104:120], sm[:, 121:122], None, ALU.mult, ALU.bypass, ["sm_ex", "sm_er"], ["afft"])
                    CP("dve", h2a[:, 1025:1041], afft[:], ["afft"], ["h2a_aff"])
                    tokbase = i * 128
                    P.op("pool", lambda e: e.iota(tidi[:], pattern=[[0, 1]], base=tokbase, channel_multiplier=1), [], ["tidi"])
                    CP("dve", h2a[:, 1024:1025], tidi[:], ["tidi"], ["h2a_id"])
                    r0 = i * 128 if kind == "l" else T + i * 128
                    DMA("sp", h2_d[r0:r0 + 128, :], h2a[:], ["h2a", "h2a_aff", "h2a_id"], ["h2_d"])
                    DMA("sp", aff_d[r0:r0 + 128, :], afft[:], ["afft"], ["aff_d"])

                def groups(lst):
                    gs, curg = [], []
                    for it in lst:
                        if it is None:
                            if curg:
                                gs.append(curg)
                            curg = []
                        else:
                            curg.append(it)
                    if curg:
                        gs.append(curg)
                    return gs

                def merge(a, b):
                    ga, gb = groups(a), groups(b)
                    out_, ia, ib = [], 0, 0
                    na, nb_ = len(ga), len(gb)
                    while ia < na or ib < nb_:
                        if ib >= nb_ or (ia < na and ia * nb_ <= ib * na):
                            out_ += ga[ia]
                            ia += 1
                        else:
                            out_ += gb[ib]
                            ib += 1
                    return out_

                for j in range(NCT):
                    project("c", j)
                if l == 0:
                    for j in range(NCT):
                        heads("c", j, [])
                        post("c", j)
                P.barrier()
                for i in range(min(W + 1, NT)):
                    project("l", i)
                for i in range(NT):
                    bgA, bgB = [], []
                    if i + W + 1 < NT:
                        if l == 1 and i == 0:
                            project("l", i + W + 1)
                        else:
                            P.defer_begin()
                            project("l", i + W + 1)
                            bgA = P.defer_end()
                    if i >= 1 and DEFER_POST:
                        P.defer_begin()
                        post("l", i - 1, DEFER_POST)
                        bgB = P.defer_end()
                    heads("l", i, merge(bgA, bgB))
                    if i >= 1 and (DEFER_POST & 4) and STORE_FG:
                        post_store("l", i - 1)
                    if i >= 1 and DEFER_POST and DEFER_POST != 7:
                        post("l", i - 1, 7 - DEFER_POST)
                    if not DEFER_POST:
                        post("l", i)
                if DEFER_POST:
                    post("l", NT - 1)
                P.barrier()
                P.flush()

        def moe(l, modc):
            with_ctx = (l == 0)
            ntl = NT + (NCT if with_ctx else 0)
            with contextlib.ExitStack() as es:
                dsti = sbuf(es, "dsti", [128, NT + NCT, NE], I32)
                g2rep = modrep[:, 5, :]
                with contextlib.ExitStack() as es2:
                    afftm = sbuf(es2, "afftm", [128, NT + NCT, NE], F32)
                    affT = sbuf(es2, "affT", [NE, T + CT], F32)
                    wk = sbuf(es2, "wk", [NE, T + CT], F32)
                    wk2 = sbuf(es2, "wk2", [NE, T + CT], F32)
                    st = sbuf(es2, "st", [NE, 32], F32)
                    DMA("sp", afftm[:, 0:ntl, :], aff_d[0:ntl * 128, :].rearrange("(n p) e -> p n e", p=128), ["aff_d"], ["afftm"])
                    for n in range(ntl):
                        pw = psW[n % 2]
                        TR(pw[0:NE, 0:128], afftm[:, n, :], identf[:], ["afftm", "identf"], [f"psW{n % 2}"])
                        CP("act" if n % 2 else "dve", affT[:, n * 128:(n + 1) * 128], pw[0:NE, 0:128], [f"psW{n % 2}"], [f"affT{n // NT}"])
                    sets = [(0, T, cap, 0, 0)]
                    if with_ctx:
                        sets.append((T, T + CT, CCAP, cap, 1))
                    for (t0, t1, cp_, slot0, si) in sets:
                        b = si * 8
                        lo, hi, mid, cnt, ge, d1 = [st[:, b + j:b + j + 1] for j in range(6)]
                        kk = f"st{si}"
                        MEMSET("dve", lo, 0.0, [kk])
                        MEMSET("dve", hi, 1.0, [kk])
                        MEMSET("dve", mid, 0.5, [kk])
                        for it in range(30):
                            TS("dve", wk[:, t0:t1], affT[:, t0:t1], mid, None, ALU.is_gt, ALU.add, [f"affT{si}", kk], [f"wk{si}", kk], accum=cnt)
                            TS("dve", ge, cnt, cp_ - 0.5, None, ALU.is_gt, ALU.bypass, [kk], [kk])
                            TT("dve", d1, mid, lo, ALU.subtract, [kk], [kk])
                            STT("dve", lo, d1, ge, lo, ALU.mult, ALU.add, [kk], [kk])
                            TT("dve", d1, hi, mid, ALU.subtract, [kk], [kk])
                            STT("dve", hi, d1, ge, mid, ALU.mult, ALU.add, [kk], [kk])
                            STT("dve", mid, lo, 1.0, hi, ALU.mult, ALU.add, [kk], [kk])
                            TS("dve", mid, mid, 0.5, None, ALU.mult, ALU.bypass, [kk], [kk])
                        TS("dve", wk[:, t0:t1], affT[:, t0:t1], lo, None, ALU.is_gt, ALU.bypass, [f"affT{si}", kk], [f"wk{si}"])
                        P.op("dve", lambda e, t0=t0, t1=t1: e.tensor_tensor_scan(out=wk2[:, t0:t1], data0=wk[:, t0:t1], data1=wk[:, t0:t1],
                                                                                 initial=0.0, op0=ALU.add, op1=ALU.max),
                             [f"wk{si}"], [f"wk2{si}"])
                        TS("dve", affT[:, t0:t1], wk2[:, t0:t1], cp_ + 0.5, None, ALU.is_lt, ALU.bypass, [f"wk2{si}"], [f"affT{si}"])
                        TT("dve", wk[:, t0:t1], wk[:, t0:t1], affT[:, t0:t1], ALU.mult, [f"wk{si}", f"affT{si}"], [f"wk{si}"])
                        TS("dve", wk2[:, t0:t1], wk2[:, t0:t1], BIG - 1.0 + slot0, None, ALU.add, ALU.bypass, [f"wk2{si}"], [f"wk2{si}"])
                        STT("dve", wk2[:, t0:t1], wk[:, t0:t1], -BIG, wk2[:, t0:t1], ALU.mult, ALU.add, [f"wk{si}", f"wk2{si}"], [f"wk2{si}"])
                    for n in range(ntl):
                        pw = psW[n % 2]
                        si = n // NT
                        TR(pw[:, 0:NE], wk2[:, n * 128:(n + 1) * 128], identf[0:NE, 0:NE], [f"wk2{si}", "identf"], [f"psW{n % 2}"])
                        CP("dve", dsti[:, n, :], pw[:, 0:NE], [f"psW{n % 2}"], ["dsti"])
                    P.barrier()
                    P.flush()
                with contextlib.ExitStack() as es3:
                    NHR = 4
                    hrow = [sbuf(es3, f"hrow{i}", [128, ROWW], F32) for i in range(NHR)]
                    xgT = sbuf(es3, "xgT", [128, KC, cap + CCAP], BF16)
                    hTt = sbuf(es3, "hTt", [128, 16, cap + CCAP], BF16)
                    wgu = [sbuf(es3, f"wgu{i}", [128, 2, KC, 512], BF16) for i in range(2)]
                    wdn = sbuf(es3, "wdn", [128, 16, D], BF16)
                    xgc = [sbuf(es3, f"xgc{i}", [128, ROWW], F32) for i in range(2)]
                    xgb = [sbuf(es3, f"xgb{i}", [128, D], BF16) for i in range(2)]
                    yv = [sbuf(es3, f"yv{i}", [128, D], F32) for i in range(2)]
                    sil = [sbuf(es3, f"sil{i}", [128, 512], F32) for i in range(2)]
                    tokg = [sbuf(es3, f"tokg{i}", [128, 16], I32) for i in range(2)]
                    gatg = [sbuf(es3, f"gatg{i}", [128, 16], F32) for i in range(2)]
                    NG = 4
                    EPG = NE // NG
                    hcnt = [0]

                    def dispatch_ops(g):
                        P.defer_begin()
                        slots = []

                        def load(n):
                            b_ = hcnt[0] % NHR
                            hcnt[0] += 1
                            slots.append(b_)
                            DMA("sp", hrow[b_][:], h2_d[n * 128:(n + 1) * 128, :], ["h2_d"], [f"hrow{b_}"])

                        for n in range(min(NHR - 1, ntl)):
                            load(n)
                        for n in range(ntl):
                            b_ = slots[n]
                            hr = hrow[b_]
                            for e_ in range(g * EPG, (g + 1) * EPG):
                                P.op("pool", lambda e, hr=hr, n=n, e_=e_: e.indirect_dma_start(
                                    out=xg_d[e_], out_offset=bass.IndirectOffsetOnAxis(ap=dsti[:, n, e_:e_ + 1], axis=0),
                                    in_=hr[:], in_offset=None, bounds_check=bc(e, XR - 1), oob_is_err=False),
                                    [f"hrow{b_}", "dsti"], [f"xg{e_}_{n}"], dma=True)
                            if n + NHR - 1 < ntl:
                                load(n + NHR - 1)
                        return P.defer_end()

                    nlc = cap // 128
                    tokchunks = [(c * 128, 128) for c in range(nlc)]
                    if with_ctx:
                        tokchunks.append((cap, CCAP))
                    ntok = cap + (CCAP if with_ctx else 0)
                    ncols = []
                    c0 = 0
                    while c0 < ntok:
                        ncols.append((c0, min(512, ntok - c0)))
                        c0 += 512
                    nyv = 0
                    unit = 0

                    def load_unit(e_, q):
                        nonlocal unit
                        u = unit % 2
                        unit += 1
                        for k in range(KC):
                            DMA("pool", wgu[u][:, 0, k, :], wg_d[l, e_, k * 128:(k + 1) * 128, q * 512:(q + 1) * 512], [], [f"wgu{u}g{k}"])
                            DMA("pool", wgu[u][:, 1, k, :], wu_d[l, e_, k * 128:(k + 1) * 128, q * 512:(q + 1) * 512], [], [f"wgu{u}u{k}"])
                        return u

                    def load_wd(e_):
                        for f in range(16):
                            DMA("pool", wdn[:, f, :], wd_d[l, e_, f * 128:(f + 1) * 128, :], [], [f"wdn{f}"])

                    def gather_T(e_, only=None, part=3):
                        pe_ = e_ % 2
                        for ci, (r0, nr) in enumerate(tokchunks):
                            if only is not None and ci != only:
                                continue
                            xc_ = xgc[ci % 2]
                            xb_ = xgb[ci % 2]
                            if part & 1:
                                DMA("sp", xc_[0:nr, :], xg_d[e_][r0:r0 + nr, :], [f"xg{e_}_{n}" for n in range(ntl)], [f"xgc{ci % 2}"])
                                CP("dve", xb_[0:nr, :], xc_[0:nr, 0:D], [f"xgc{ci % 2}"], [f"xgb{ci % 2}"])
                                CP("pool", tokg[pe_][0:nr, ci:ci + 1], xc_[0:nr, 1024:1025], [f"xgc{ci % 2}"], [f"tokg{pe_}"])
                                CP("pool", gatg[pe_][0:nr, ci:ci + 1], xc_[0:nr, 1025 + e_:1026 + e_], [f"xgc{ci % 2}"], [f"gatg{pe_}"])
                            if part & 2:
                                for k in range(KC):
                                    TR(psT[:, k, 0:nr], xb_[0:nr, k * 128:(k + 1) * 128], ident[0:nr, 0:nr], [f"xgb{ci % 2}", "ident"], ["psT"])
                                CP("act", xgT[:, :, r0:r0 + nr], psT[:, :, 0:nr], ["psT"], ["xgT"])

                    git = [0]

                    def gateup(e_, q, u):
                        for fi in range(4):
                            f = q * 4 + fi
                            for (n0, nn) in ncols:
                                s = git[0] % 2
                                git[0] += 1
                                G, U = (psS[0], psS[1]) if s == 0 else (psW[0], psW[1])
                                gk, uk = ("psS0", "psS1") if s == 0 else ("psW0", "psW1")
                                for k in range(KC):
                                    MM(G[:, 0:nn], wgu[u][:, 0, k, fi * 128:(fi + 1) * 128], xgT[:, k, n0:n0 + nn], k == 0, k == KC - 1,
                                       [f"wgu{u}g{k}", "xgT"], [gk])
                                for k in range(KC):
                                    MM(U[:, 0:nn], wgu[u][:, 1, k, fi * 128:(fi + 1) * 128], xgT[:, k, n0:n0 + nn], k == 0, k == KC - 1,
                                       [f"wgu{u}u{k}", "xgT"], [uk])
                                s_ = sil[s]
                                sk = f"sil{s}"
                                ACT(s_[:, 0:nn], G[:, 0:nn], AF.Silu, [gk], [sk])
                                TT("dve", hTt[:, f, n0:n0 + nn], s_[:, 0:nn], U[:, 0:nn], ALU.mult, [sk, uk], [f"hTt{f}"])

                    def down(e_, only=None):
                        nonlocal nyv
                        pe_ = e_ % 2
                        for ci, (r0, nr) in enumerate(tokchunks):
                            if only is not None and ci != only:
                                continue
                            y_ = yv[nyv % 2]
                            yk = f"yv{nyv % 2}"
                            nyv += 1
                            is_ctx = (r0 >= cap)
                            gsrc = modc[:, 5, :] if is_ctx else g2rep
                            for j in range(2):
                                for f in range(16):
                                    MM(psO[j][0:nr, :], hTt[:, f, r0:r0 + nr], wdn[:, f, j * 512:(j + 1) * 512], f == 0, f == 15,
                                       [f"hTt{f}", f"wdn{f}"], [f"psO{j}"])
                                STT("dve", y_[0:nr, j * 512:(j + 1) * 512], psO[j][0:nr, :], gatg[pe_][0:nr, ci:ci + 1],
                                    gsrc[0:nr, j * 512:(j + 1) * 512], ALU.mult, ALU.mult, [f"psO{j}", f"gatg{pe_}", "mod5", "modc5"], [yk])
                            dst = xc_d if is_ctx else out
                            wkeys = [f"cmb{e_}_{ci}"]
                            rkeys = [f"cmb{e_ - 1}_{cj}" for cj in range(len(tokchunks))]
                            P.op("pool", lambda e, y_=y_, nr=nr, dst=dst, pe_=pe_, ci=ci, is_ctx=is_ctx: e.indirect_dma_start(
                                out=dst, out_offset=bass.IndirectOffsetOnAxis(ap=tokg[pe_][0:nr, ci:ci + 1], axis=0),
                                in_=y_[0:nr, :], in_offset=None, bounds_check=bc(e, (CT if is_ctx else T) - 1), oob_is_err=True,
                                compute_op=ALU.add), [yk, f"tokg{pe_}"] + rkeys, wkeys, dma=True)

                    us = {}
                    us[(0, 0)] = load_unit(0, 0)
                    us[(0, 1)] = load_unit(0, 1)
                    P.replay(dispatch_ops(0))
                    gather_T(0)
                    bg = []
                    bgi = 0
                    for e_ in range(NE):
                        g = e_ // EPG
                        if e_ % EPG == 0:
                            if bgi < len(bg):
                                P.replay(bg[bgi:])
                            bg = dispatch_ops(g + 1) if g + 1 < NG else []
                            bgi = 0
                        per = (len(bg) + 4 * EPG - 1) // (4 * EPG) if bg else 0
                        load_wd(e_)
                        for q in range(4):
                            gateup(e_, q, us[(e_, q)])
                            if bgi < len(bg):
                                P.replay(bg[bgi:bgi + per])
                                bgi += per
                            nq = q + 2
                            if nq < 4:
                                us[(e_, nq)] = load_unit(e_, nq)
                            elif e_ + 1 < NE:
                                us[(e_ + 1, nq - 4)] = load_unit(e_ + 1, nq - 4)
                        nchk = len(tokchunks)
                        if e_ + 1 < NE:
                            gather_T(e_ + 1, 0, 1)
                        for ci in range(nchk):
                            down(e_, ci)
                            if e_ + 1 < NE:
                                gather_T(e_ + 1, ci, 2)
                                if ci + 1 < nchk:
                                    gather_T(e_ + 1, ci + 1, 1)
                    P.barrier()
                    P.flush()

        for l in range(n_layers):
            with contextlib.ExitStack() as les:
                nvc = 6 if l == 0 else 2
                modc = sbuf(les, "modc", [128, nvc, D], F32)
                modulation2(l, modc, nvc)
                attention(l, modc)
                if not SKIP_MOE:
                    moe(l, modc)
        P.barrier()
        P.flush(final=True)
    return nc


def _rope_tables(T):
    pos = np.arange(T)
    row = (pos // 64).astype(np.float32)
    col = (pos % 64).astype(np.float32)
    inv = (np.float32(10000.0) ** (-np.arange(16, dtype=np.float32) / np.float32(16))).astype(np.float32)
    ar = (row[:, None] * inv).astype(np.float32)
    ac = (col[:, None] * inv).astype(np.float32)
    C = np.concatenate([np.cos(ar), np.cos(ar), np.cos(ac), np.cos(ac)], 1).astype(np.float32)
    S = np.concatenate([-np.sin(ar), np.sin(ar), -np.sin(ac), np.sin(ac)], 1).astype(np.float32)
    return np.ascontiguousarray(C), np.ascontiguousarray(S)


def _mask0():
    kk = np.arange(128)[:, None]
    a = np.arange(128)[None, :]
    m = np.stack([(kk >= a), np.ones((128, 128), bool), (kk <= a)], 1)
    return np.ascontiguousarray(m.reshape(128, 384).astype(np.float32))


def _bias_tab(rpb, T):
    NT = T // 128
    R = T // 64
    reps = [0, 1, 2, NT - 2, NT - 1]
    kk = np.arange(128)
    qq = np.arange(128)
    tab = np.full((5, 128, 16, 5, 128), NEGM, np.float32)
    for v, i in enumerate(reps):
        qrow = 2 * i + qq // 64
        qcol = qq % 64
        rs = np.clip(qrow - 4, 0, R - 8)
        ws = np.clip(qcol - 8, 0, 48)
        dlo_v = [0, -1, -2, -2, -3][v]
        for s in range(5):
            dl = dlo_v + s
            j = i + dl
            if j < 0 or j >= NT:
                continue
            krow = 2 * j + kk // 64
            kcol = kk % 64
            valid = ((krow[:, None] >= rs[None, :]) & (krow[:, None] < rs[None, :] + 8)
                     & (kcol[:, None] >= ws[None, :]) & (kcol[:, None] < ws[None, :] + 16))
            dr = np.clip(krow[:, None] - qrow[None, :] + 7, 0, 14)
            dc = np.clip(kcol[:, None] - qcol[None, :], -15, 15) + 15
            b = rpb[:, dr, dc]
            b = np.where(valid[None], b, NEGM)
            tab[v, :, :, s, :] = np.transpose(b, (1, 0, 2))
    return np.ascontiguousarray(tab.reshape(5, 128, 16 * 5 * 128))


def _lay(v):
    return np.ascontiguousarray(np.asarray(v, np.float32).reshape(KC, 128).T)


_NC_CACHE = {}


def make_in_maps(inp, T):
    f = lambda a: np.ascontiguousarray(np.asarray(a, np.float32))
    B = inp["x"].shape[0]
    C, S = _rope_tables(T)
    shared = dict(
        w_mod=f(inp["w_mod"]), b_mod=f(inp["b_mod"]), norm_mix=f(inp["norm_mix"]), norm_ffn=f(inp["norm_ffn"]),
        a_w_qkv=f(inp["a_w_qkv"][0]), a_q_gain=f(inp["a_q_gain"][0]), a_k_gain=f(inp["a_k_gain"][0]),
        a_sink=f(inp["a_sink"][0]), a_w_o=f(inp["a_w_o"][0]),
        b_w_qkv=f(inp["b_w_qkv"][0]), b_q_gain=f(inp["b_q_gain"][0]), b_k_gain=f(inp["b_k_gain"][0]),
        b_w_o=f(inp["b_w_o"][0]), bias_tab=_bias_tab(f(inp["b_rpb"][0]), T), mask0=_mask0(),
        ropeC=C, ropeS=S, moe_router=f(inp["moe_router"]), moe_w_gate=f(inp["moe_w_gate"]),
        moe_w_up=f(inp["moe_w_up"]), moe_w_down=f(inp["moe_w_down"]),
    )
    maps = []
    for b in range(B):
        m = dict(shared)
        m["x"] = f(inp["x"][b])
        m["ctx"] = f(inp["ctx"][b])
        m["cvec"] = np.ascontiguousarray(np.stack([_lay(inp["c"][b]), _lay(inp["c_ctx"])], 0))
        maps.append(m)
    return maps


def kernel(**inputs):
    T = inputs["x"].shape[1]
    B = inputs["x"].shape[0]
    if T not in _NC_CACHE:
        _NC_CACHE[T] = build(T)
    nc = _NC_CACHE[T]
    maps = make_in_maps(inputs, T)
    res = run_bass_kernel_spmd(nc, maps, core_ids=list(range(B)))
    return np.stack([np.asarray(r["out"], np.float32) for r in res.results], 0)
```
